# Optimizing a Trainium2 kernel written in Bass

```python
import math
import jax, jax.numpy as jnp
from jax import lax
import numpy as np

D_MODEL = 1024
BATCH = 16
SEQ = 2048
DEPTH = 1

D_MIX = D_MODEL
SB_HEADS = 8
SB_HEAD_DIM = 64
SB_WIDTH = SB_HEADS * SB_HEAD_DIM
RW_HEADS = 8
RW_HEAD_DIM = 64
RW_WIDTH = RW_HEADS * RW_HEAD_DIM
LORA_W = 64
LORA_A = 64
LORA_G = 128
RW_IN = 3 * RW_WIDTH + LORA_W + LORA_A + LORA_G
SB_IN = 3 * SB_WIDTH
N_IN = SB_IN + RW_IN
Q_BLOCK = 128
N_GROUPS = 4
EXPERTS_PER_GROUP = 8
N_EXPERTS = N_GROUPS * EXPERTS_PER_GROUP
TOP_K_IN_GROUP = 2
D_EXPERT = 256
RMS_EPS = 1e-6
GN_EPS = 64e-5

kernel_name = "hybrid_sbattn_rwkv7_hiermoe"


def rmsnorm(x, g):
    xf = x.astype(jnp.float32)
    y = xf * lax.rsqrt(jnp.mean(xf * xf, axis=-1, keepdims=True) + RMS_EPS)
    return (y * g.astype(jnp.float32)).astype(x.dtype)


def token_shift(u):
    return jnp.pad(u, ((0, 0), (1, 0), (0, 0)))[:, :-1]


def stick_breaking_attention(q, k, v):
    B, H, S, dh = q.shape
    scale = 1.0 / math.sqrt(dh)
    outs = []
    for blk in range(S // Q_BLOCK):
        t0 = blk * Q_BLOCK
        kv_len = t0 + Q_BLOCK
        qb = q[:, :, t0:kv_len].astype(jnp.float32)
        kb = k[:, :, :kv_len].astype(jnp.float32)
        vb = v[:, :, :kv_len].astype(jnp.float32)
        z = jnp.einsum('bhqd,bhkd->bhqk', qb, kb) * scale
        t_pos = t0 + jnp.arange(Q_BLOCK)[:, None]
        s_pos = jnp.arange(kv_len)[None, :]
        strict = s_pos < t_pos
        log_rest = jnp.where(strict, jax.nn.log_sigmoid(-z), 0.0)
        suffix = lax.cumsum(log_rest, axis=3, reverse=True) - log_rest
        weights = jnp.where(strict, jnp.exp(jax.nn.log_sigmoid(z) + suffix), 0.0)
        outs.append(jnp.einsum('bhqk,bhkd->bhqd', weights, vb))
    return jnp.concatenate(outs, axis=2)


def rwkv7_scan(r, w, k, v, kk, a):
    B, S, H, N = r.shape
    xs = tuple(jnp.moveaxis(t, 1, 0) for t in (r, w, k, v, kk, a))

    def step(state, inp):
        r_t, w_t, k_t, v_t, kk_t, a_t = inp
        sa = jnp.einsum('bhij,bhj->bhi', state, -kk_t)
        state = (state * w_t[:, :, None, :]
                 + sa[..., None] * (kk_t * a_t)[:, :, None, :]
                 + v_t[..., None] * k_t[:, :, None, :])
        out = jnp.einsum('bhij,bhj->bhi', state, r_t)
        return state, out

    state0 = jnp.zeros((B, H, N, N), jnp.float32)
    _, outs = lax.scan(step, state0, xs)
    return jnp.moveaxis(outs, 0, 1)


def rwkv7_group(u_rw, shift_mu, w0, w2, a0, a2, g2, k_k, k_a, r_k, ln_w, ln_b):
    B, S, _ = u_rw.shape
    uf = u_rw.astype(jnp.float32)
    um = uf + (token_shift(uf) - uf) * shift_mu.astype(jnp.float32)
    o = 0
    r = um[..., o:o + RW_WIDTH]; o += RW_WIDTH
    k = um[..., o:o + RW_WIDTH]; o += RW_WIDTH
    v = um[..., o:o + RW_WIDTH]; o += RW_WIDTH
    xw = um[..., o:o + LORA_W]; o += LORA_W
    xa = um[..., o:o + LORA_A]; o += LORA_A
    xg = um[..., o:o + LORA_G]
    w_log = -jax.nn.softplus(-(w0 + jnp.tanh(xw) @ w2)) - 0.5
    decay = jnp.exp(-jnp.exp(w_log))
    a = jax.nn.sigmoid(a0 + xa @ a2)
    g = jax.nn.sigmoid(xg) @ g2
    hs = lambda t: t.reshape(B, S, RW_HEADS, RW_HEAD_DIM)
    kk = hs(k * k_k)
    kk = kk * lax.rsqrt(jnp.maximum(jnp.sum(kk * kk, axis=-1, keepdims=True), 1e-24))
    k = k * (1.0 + (a - 1.0) * k_a)
    r_h, k_h, v_h, w_h, a_h = hs(r), hs(k), hs(v), hs(decay), hs(a)
    out = rwkv7_scan(r_h, w_h, k_h, v_h, kk, a_h)
    mean = jnp.mean(out, axis=-1, keepdims=True)
    var = jnp.mean(jnp.square(out - mean), axis=-1, keepdims=True)
    out = ((out - mean) * lax.rsqrt(var + GN_EPS)).reshape(B, S, RW_WIDTH) * ln_w + ln_b
    bonus = jnp.sum(r_h * k_h * r_k, axis=-1, keepdims=True) * v_h
    out = (out + bonus.reshape(B, S, RW_WIDTH)) * g
    return out.astype(u_rw.dtype)


def sb_group(u_sb, sb_out_g):
    B, S, _ = u_sb.shape
    heads = lambda t: t.reshape(B, S, SB_HEADS, SB_HEAD_DIM).transpose(0, 2, 1, 3)
    q = heads(u_sb[..., :SB_WIDTH])
    k = heads(u_sb[..., SB_WIDTH:2 * SB_WIDTH])
    v = heads(u_sb[..., 2 * SB_WIDTH:])
    o = stick_breaking_attention(q, k, v).transpose(0, 2, 1, 3)
    o = o * lax.rsqrt(jnp.mean(o * o, axis=-1, keepdims=True) + RMS_EPS)
    return (o.reshape(B, S, SB_WIDTH) * sb_out_g).astype(u_sb.dtype)


def hier_moe(x2d, w_grp, b_grp, w_exp, b_exp, w_gate, w_up, w_down):
    T = x2d.shape[0]
    xf = x2d.astype(jnp.float32)
    grp_prob = jax.nn.softmax(xf @ w_grp.astype(jnp.float32) + b_grp.astype(jnp.float32), axis=-1)
    g_val, g_idx = lax.top_k(grp_prob, 1)
    exp_logits = (xf @ w_exp.astype(jnp.float32) + b_exp.astype(jnp.float32)).reshape(T, N_GROUPS, EXPERTS_PER_GROUP)
    sel = jnp.take_along_axis(exp_logits, g_idx[:, :, None], axis=1)[:, 0]
    e_val, e_idx = lax.top_k(jax.nn.softmax(sel, axis=-1), TOP_K_IN_GROUP)
    weights = g_val * e_val / jnp.sum(e_val, axis=-1, keepdims=True)
    global_idx = g_idx * EXPERTS_PER_GROUP + e_idx
    combine = jnp.sum(jax.nn.one_hot(global_idx, N_EXPERTS, dtype=jnp.float32) * weights[..., None], axis=1)
    combine = combine.astype(x2d.dtype)
    y = jnp.zeros_like(x2d)
    for e in range(N_EXPERTS):
        h = jax.nn.silu(x2d @ w_gate[e]) * (x2d @ w_up[e])
        y = y + combine[:, e:e + 1] * (h @ w_down[e])
    return y


def setup_inputs(seed: int = 0) -> dict:
    key = jax.random.key(seed)
    ks = jax.random.split(key, 28)
    f32 = jnp.float32
    nrm = lambda k, shape, s: jax.random.normal(k, shape, f32) * s
    L = DEPTH
    return {
        "x": jax.random.normal(ks[0], (BATCH, SEQ, D_MODEL), f32),
        "norm_mix_g": 1.0 + nrm(ks[1], (L, D_MODEL), 0.02),
        "w_in": nrm(ks[2], (L, D_MODEL, N_IN), D_MODEL ** -0.5),
        "shift_mu": jax.random.uniform(ks[3], (L, RW_IN), f32, 0.1, 0.9),
        "sb_out_g": 1.0 + nrm(ks[4], (L, SB_WIDTH), 0.02),
        "rw_w0": jax.random.uniform(ks[5], (L, RW_WIDTH), f32, -6.5, -1.5),
        "rw_w2": nrm(ks[6], (L, LORA_W, RW_WIDTH), 0.5 * LORA_W ** -0.5),
        "rw_a0": nrm(ks[7], (L, RW_WIDTH), 0.1),
        "rw_a2": nrm(ks[8], (L, LORA_A, RW_WIDTH), LORA_A ** -0.5),
        "rw_g2": nrm(ks[9], (L, LORA_G, RW_WIDTH), LORA_G ** -0.5),
        "rw_k_k": 0.85 + nrm(ks[10], (L, RW_WIDTH), 0.02),
        "rw_k_a": 1.0 + nrm(ks[11], (L, RW_WIDTH), 0.02),
        "rw_r_k": nrm(ks[12], (L, RW_HEADS, RW_HEAD_DIM), 0.1),
        "rw_ln_w": 1.0 + nrm(ks[13], (L, RW_WIDTH), 0.02),
        "rw_ln_b": nrm(ks[14], (L, RW_WIDTH), 0.02),
        "w_out": nrm(ks[15], (L, D_MIX, D_MODEL), D_MIX ** -0.5),
        "norm_ffn_g": 1.0 + nrm(ks[16], (L, D_MODEL), 0.02),
        "router_grp_w": nrm(ks[17], (L, D_MODEL, N_GROUPS), D_MODEL ** -0.5),
        "router_grp_b": nrm(ks[18], (L, N_GROUPS), 0.01),
        "router_exp_w": nrm(ks[19], (L, D_MODEL, N_EXPERTS), D_MODEL ** -0.5),
        "router_exp_b": nrm(ks[20], (L, N_EXPERTS), 0.01),
        "exp_w_gate": nrm(ks[21], (L, N_EXPERTS, D_MODEL, D_EXPERT), D_MODEL ** -0.5),
        "exp_w_up": nrm(ks[22], (L, N_EXPERTS, D_MODEL, D_EXPERT), D_MODEL ** -0.5),
        "exp_w_down": nrm(ks[23], (L, N_EXPERTS, D_EXPERT, D_MODEL), D_EXPERT ** -0.5),
        "final_norm_g": 1.0 + nrm(ks[24], (D_MODEL,), 0.02),
    }


def reference(x, norm_mix_g, w_in, shift_mu, sb_out_g, rw_w0, rw_w2, rw_a0, rw_a2, rw_g2,
              rw_k_k, rw_k_a, rw_r_k, rw_ln_w, rw_ln_b, w_out, norm_ffn_g,
              router_grp_w, router_grp_b, router_exp_w, router_exp_b,
              exp_w_gate, exp_w_up, exp_w_down, final_norm_g):
    B, S, D = x.shape
    h = x
    for l in range(DEPTH):
        u = rmsnorm(h, norm_mix_g[l]) @ w_in[l]
        o_sb = sb_group(u[..., :SB_IN], sb_out_g[l])
        o_rw = rwkv7_group(u[..., SB_IN:], shift_mu[l], rw_w0[l], rw_w2[l], rw_a0[l], rw_a2[l],
                           rw_g2[l], rw_k_k[l], rw_k_a[l], rw_r_k[l], rw_ln_w[l], rw_ln_b[l])
        h = h + jnp.concatenate([o_sb, o_rw], axis=-1) @ w_out[l]
        xn = rmsnorm(h, norm_ffn_g[l]).reshape(B * S, D)
        y = hier_moe(xn, router_grp_w[l], router_grp_b[l], router_exp_w[l], router_exp_b[l],
                     exp_w_gate[l], exp_w_up[l], exp_w_down[l])
        h = h + y.reshape(B, S, D)
    return rmsnorm(h, final_norm_g)
```

```python
import numpy as np
import ml_dtypes
import concourse.bass as bass
import concourse.mybir as mybir
from concourse.bass_utils import run_bass_kernel_spmd

F32 = mybir.dt.float32
BF16 = mybir.dt.bfloat16
AF = mybir.ActivationFunctionType
ALU = mybir.AluOpType
AX = mybir.AxisListType

PK_GMIX, PK_SBG, PK_MUL, PK_ELAST, PK_ID, PK_SU, PK_IN, PK_LO, PK_NT, NPK = 0, 8, 12, 14, 16, 144, 272, 400, 528, 656
BC_MU, BC_W0, BC_A0, BC_KK, BC_KA, BC_RK, BC_LNW, BC_LNB, BC_GFFN, BC_RB, BC_GFIN, NBC = (
    0, 1536, 2048, 2560, 3072, 3584, 4096, 4608, 5120, 6144, 6192, 7216)
BC_MIX = 6192


import os as _osmod
STRICT = bool(_osmod.environ.get("KSTRICT"))


class Tok:
    __slots__ = ("last_w", "readers")

    def __init__(self):
        self.last_w = None
        self.readers = []


class Tile:
    def __init__(self, ap, toks=None):
        self.ap = ap
        self.toks = toks if toks is not None else [Tok()]

    def __getitem__(self, k):
        return self.ap[k]


class Prog:
    ENGS = ("pe", "act", "dve", "pool", "sp")
    NDMA = 40

    def __init__(self, nc):
        self.nc = nc
        self.ops = {e: [] for e in self.ENGS}
        self.waited = {e: {} for e in self.ENGS}
        self.ndma = 0
        self.dma_last = [None] * self.NDMA
        self.final_dma = []
        self.last_compute = {e: None for e in self.ENGS}

    def _need(self, eng, ref, waits):
        kind, key, val = ref
        w = self.waited[eng]
        k = (kind, key)
        if w.get(k, -1) >= val:
            return
        w[k] = val
        waits.append(ref)
        if kind == "eng":
            self.ops[key][val]["signal"] = True

    def add(self, eng, fn, reads=(), writes=(), dma=False, final=False):
        import os
        self.nadd = getattr(self, "nadd", 0) + 1
        if self.nadd > int(os.environ.get("KSTOP", "100000000")) or getattr(self, "stopped", False):
            return None
        reads = [k for t in reads for k in t.toks]
        writes = [k for t in writes for k in t.toks]
        deps = []
        for t in reads:
            if t.last_w is not None:
                deps.append((t.last_w, True))
        for t in writes:
            if t.last_w is not None:
                deps.append((t.last_w, False))
            for r in t.readers:
                deps.append((r, False))
        waits = []
        for ref, raw in deps:
            kind, key, val = ref
            if kind == "eng" and key == eng and not dma:
                if eng == "pe" or (not raw and not STRICT):
                    continue
            self._need(eng, ref, waits)
        idx = len(self.ops[eng])
        op = {"fn": fn, "waits": waits, "signal": False, "dma": None}
        if dma:
            slot = self.ndma % self.NDMA
            val = 16 * (self.ndma // self.NDMA + 1)
            self.ndma += 1
            if self.dma_last[slot] is not None:
                self._need(eng, self.dma_last[slot], waits)
            myref = ("dma", slot, val)
            self.dma_last[slot] = myref
            op["dma"] = (slot, val)
            if final:
                self.final_dma.append(myref)
        else:
            myref = ("eng", eng, idx)
            self.last_compute[eng] = myref
        self.ops[eng].append(op)
        for t in reads:
            t.readers.append(myref)
        for t in writes:
            t.last_w = myref
            t.readers = []
        return myref

    def barrier(self):
        refs = [r for r in self.last_compute.values() if r is not None]
        refs += [r for r in self.dma_last if r is not None]
        for e in self.ENGS:
            waits = []
            for ref in refs:
                if ref[0] == "eng" and ref[1] == e:
                    continue
                self._need(e, ref, waits)
            if waits:
                self.ops[e].append({"fn": None, "waits": waits, "signal": False, "dma": None})

    def emit(self):
        nc = self.nc
        fw = []
        for ref in self.final_dma:
            self._need("sp", ref, fw)
        if fw:
            self.ops["sp"].append({"fn": None, "waits": fw, "signal": False, "dma": None})
        ordinal = {}
        for e in self.ENGS:
            c = 0
            for i, op in enumerate(self.ops[e]):
                if op["signal"]:
                    c += 1
                    ordinal[(e, i)] = c
            print("SEMMAX", e, c, flush=True)
        from contextlib import ExitStack
        with ExitStack() as st:
            esem = {e: st.enter_context(nc.semaphore(f"s_{e}")) for e in self.ENGS}
            dsem = [st.enter_context(nc.semaphore(f"d_{i}")) for i in range(self.NDMA)]
            block = st.enter_context(nc.Block())

            def run(e, engobj):
                for i, op in enumerate(self.ops[e]):
                    for kind, key, val in op["waits"]:
                        if kind == "eng":
                            engobj.wait_ge(esem[key], ordinal[(key, val)])
                        else:
                            engobj.wait_ge(dsem[key], val)
                    if op["fn"] is None:
                        continue
                    ins = op["fn"](engobj)
                    if op["dma"] is not None:
                        ins.then_inc(dsem[op["dma"][0]], 16)
                    elif op["signal"]:
                        ins.then_inc(esem[e], 1)

            block.tensor(lambda eng: run("pe", eng))
            block.scalar(lambda eng: run("act", eng))
            block.vector(lambda eng: run("dve", eng))
            block.gpsimd(lambda eng: run("pool", eng))
            block.sync(lambda eng: run("sp", eng))
        return {e: len(self.ops[e]) for e in self.ENGS}


class Arena:
    def __init__(self, nc, nbytes):
        self.n = nbytes // 2
        self.t = nc.alloc_sbuf_tensor("arena", [128, self.n], BF16)
        self.off = 0
        self.hi = 0

    def alloc(self, shape, dt, at=None, toks=None):
        n = int(np.prod(shape))
        el = n * (2 if dt == F32 else 1)
        el = (el + 15) // 16 * 16
        if at is None:
            assert self.off + el <= self.n, f"SBUF arena overflow {self.off + el} > {self.n}"
            at = self.off
            self.off += el
            self.hi = max(self.hi, self.off)
        ap = self.t[:, at:at + el]
        self.last_at = at
        if dt == F32:
            ap = ap.bitcast(F32)
        ap = ap[:, 0:n]
        if len(shape) == 2:
            ap = ap.rearrange("p (a b) -> p a b", a=shape[0], b=shape[1])
        elif len(shape) == 3:
            ap = ap.rearrange("p (a b c) -> p a b c", a=shape[0], b=shape[1], c=shape[2])
        return Tile(ap, toks)


def build(S, NSEQ, dbg=False):
    import os as _os
    NT = S // 128
    NTOK = S * NSEQ
    NTT = NTOK // 128
    nc = bass.Bass("TRN2", target_bir_lowering=False)
    din = lambda name, shape, dt=F32: nc.dram_tensor(name, shape, dt, kind="ExternalInput").ap()
    x = din("x", [NTOK, 1024])
    w_in = din("w_in", [1024, 3328])
    w_out = din("w_out", [1024, 1024])
    wr = din("wr", [1024, 36])
    lw = din("lw", [128, 1024])
    wg = din("wg", [32, 1024, 256])
    wu = din("wu", [32, 1024, 256])
    wd = din("wd", [32, 256, 1024])
    pk = din("pk", [128, NPK])
    bc = din("bc", [128, NBC])
    out = nc.dram_tensor("out", [NTOK, 1024], F32, kind="ExternalOutput").ap()
    hbuf = nc.dram_tensor("hbuf", [NTOK, 1024], F32).ap()
    x2d = nc.dram_tensor("x2d", [128, 8, NTOK], BF16).ap()
    dbg_t = nc.dram_tensor("dbg", [NTOK, 8192], F32, kind="ExternalOutput").ap() if dbg else None

    P = Prog(nc)
    ar = Arena(nc, 212800)
    A = ar.alloc

    pb = [Tile(nc.alloc_psum_tensor(f"pb{i}", [128, 1024], F32)[:]) for i in range(3)]
    pc = Tile(nc.alloc_psum_tensor("pc", [128, 512], F32)[:])
    pT = Tile(nc.alloc_psum_tensor("pT", [128, 1024], BF16)[:])

    pTf = Tile(pT.ap.bitcast(F32), pT.toks)

    def mm(o, lhsT, rhs, start, stop, r, w):
        P.add("pe", lambda e: e.matmul(o, lhsT=lhsT, rhs=rhs, start=start, stop=stop, skip_group_check=True), r, w)

    def tr(o, in_, ident, r, w):
        P.add("pe", lambda e: e.transpose(out=o, in_=in_, identity=ident), r, w)

    def tt(eng, o, a, b, op, r, w):
        P.add(eng, lambda e: e.tensor_tensor(out=o, in0=a, in1=b, op=op), r, w)

    def ts(eng, o, a, s1, s2, op0, op1, r, w):
        if s2 is None:
            P.add(eng, lambda e: e.tensor_scalar(out=o, in0=a, scalar1=s1, scalar2=None, op0=op0), r, w)
        else:
            P.add(eng, lambda e: e.tensor_scalar(out=o, in0=a, scalar1=s1, scalar2=s2, op0=op0, op1=op1), r, w)

    def stt(eng, o, a, s, b, op0, op1, r, w):
        P.add(eng, lambda e: e.scalar_tensor_tensor(out=o, in0=a, scalar=s, in1=b, op0=op0, op1=op1), r, w)

    def act(o, in_, func, r, w, bias=0.0, scale=1.0, accum=None):
        if accum is None:
            P.add("act", lambda e: e.activation(out=o, in_=in_, func=func, bias=bias, scale=scale), r, w)
        else:
            P.add("act", lambda e: e.activation(out=o, in_=in_, func=func, bias=bias, scale=scale, accum_out=accum), r, w)

    def cp(eng, o, in_, r, w):
        if eng == "act":
            P.add("act", lambda e: e.activation(out=o, in_=in_, func=AF.Copy), r, w)
        else:
            P.add(eng, lambda e: e.tensor_copy(out=o, in_=in_), r, w)

    def recip(o, in_, r, w):
        P.add("dve", lambda e: e.reciprocal(out=o, in_=in_), r, w)

    def red(eng, o, in_, op, r, w):
        P.add(eng, lambda e: e.tensor_reduce(out=o, in_=in_, axis=AX.X, op=op), r, w)

    def dma(eng, o, in_, r, w, final=False):
        P.add(eng, lambda e: e.dma_start(out=o, in_=in_), r, w, dma=True, final=final)

    def dump(tile, col, row0, n=512):
        if dbg:
            dma("sp", dbg_t[row0:row0 + 128, col:col + n], tile.ap, [tile], [], final=True)

    def rsqrt_small(o, in_, r, w, tmp):
        act(tmp, in_, AF.Ln, r, [tmp_tok(tmp)])
        act(o, tmp, AF.Exp, [tmp_tok(tmp)], w, scale=-0.5)

    def tmp_tok(t):
        return t

    call = A([NTT, 32], F32)
    pkt = A([NPK], F32)
    mark_moe = ar.off
    bct = A([BC_MIX], F32)
    dma("sp", pkt.ap, pk, [], [pkt])
    dma("sp", bct.ap, bc[:, 0:BC_MIX], [], [bct])
    ident_f = pkt[:, PK_ID:PK_ID + 128]
    mSU = pkt[:, PK_SU:PK_SU + 128]
    mIN = pkt[:, PK_IN:PK_IN + 128]
    mLO = pkt[:, PK_LO:PK_LO + 128]
    cb = A([4, 128], BF16)
    cp("dve", cb[:, 0, :], ident_f, [pkt], [cb])
    cp("dve", cb[:, 1, :], pkt[:, PK_NT:PK_NT + 128], [pkt], [cb])
    identb = cb[:, 0, :]
    negtri = cb[:, 1, :]
    omm = A([2], F32)
    ts("dve", omm.ap, pkt[:, PK_MUL:PK_MUL + 2], -1.0, 1.0, ALU.mult, ALU.add, [pkt], [omm])

    Wsb = A([8, 1536], BF16)
    Wrw = A([8, 1792], BF16)
    Wout = A([8, 1024], BF16)
    Wr = A([8, 36], BF16)
    lwb = A([1024], BF16)
    kT = A([4, S], BF16)
    vs = A([NT, 512], BF16)
    Hst = A([8, 64], F32)
    Hb = A([8, 64], BF16)
    mark = ar.off

    stg = [A([8, 256], F32) for _ in range(2)]
    w_in_v = w_in.rearrange("(c p) n -> p c n", p=128)
    w_out_v = w_out.rearrange("(c p) n -> p c n", p=128)
    si = 0
    for c0 in range(0, 3328, 256):
        cw = 256
        sg = stg[si % 2]
        si += 1
        dma("sp", sg[:, :, 0:cw], w_in_v[:, :, c0:c0 + cw], [], [sg])
        for c in range(8):
            eng = "dve"
            if c0 < 1536:
                dst = Wsb[:, c, c0:c0 + cw]
                dt_ = Wsb
            else:
                dst = Wrw[:, c, c0 - 1536:c0 - 1536 + cw]
                dt_ = Wrw
            if c0 < 512:
                ts(eng, dst, sg[:, c, 0:cw], pkt[:, PK_GMIX + c:PK_GMIX + c + 1], 0.125, ALU.mult, ALU.mult, [sg, pkt], [dt_])
            else:
                ts(eng, dst, sg[:, c, 0:cw], pkt[:, PK_GMIX + c:PK_GMIX + c + 1], None, ALU.mult, None, [sg, pkt], [dt_])
    for c0 in range(0, 1024, 256):
        sg = stg[si % 2]
        si += 1
        dma("sp", sg.ap, w_out_v[:, :, c0:c0 + 256], [], [sg])
        for c in range(8):
            eng = "dve"
            if c < 4:
                ts(eng, Wout[:, c, c0:c0 + 256], sg[:, c, :], pkt[:, PK_SBG + c:PK_SBG + c + 1], None, ALU.mult, None, [sg, pkt], [Wout])
            else:
                cp(eng, Wout[:, c, c0:c0 + 256], sg[:, c, :], [sg], [Wout])
    sg = stg[si % 2]
    si += 1
    dma("sp", sg[:, :, 0:36], wr.rearrange("(c p) n -> p c n", p=128), [], [sg])
    cp("dve", Wr.ap, sg[:, :, 0:36], [sg], [Wr])
    sg = stg[si % 2]
    si += 1
    dma("sp", sg[:, 0:4, :], lw.rearrange("p (a b) -> p a b", a=4), [], [sg])
    cp("dve", lwb.ap.rearrange("p (a b) -> p a b", a=4), sg[:, 0:4, :], [sg], [lwb])
    P.barrier()
    ar.off = mark

    xt = [A([1024], F32) for _ in range(2)]
    st8 = A([64], F32)
    xnb = A([1024], BF16)
    xnT = [A([8, 129], BF16) for _ in range(2)]
    qT = A([4, 128], BF16)
    S_ = []
    for _ in range(11):
        S_.append(A([512], F32))
        S_[-1].at = ar.last_at
    al2 = lambda sl, shape, dt: A(shape, dt, at=S_[sl[0]].at, toks=[k for j in sl for k in S_[j].toks])
    E2 = [al2([5, 6], [1024], F32), al2([3, 4], [1024], F32)]
    X2 = [al2([7, 8], [1024], F32), al2([9, 10], [1024], F32)]
    Lp2 = [al2([0], [1024], BF16)]
    AT2 = [al2([1], [1024], BF16)]
    acc = al2([2], [512], F32)
    xn2 = al2([7], [1024], BF16)
    E_ = E2[0]
    Atc = A([512], BF16)
    At = A([8, 128], BF16)
    Bt = A([512], BF16)
    Kt = A([512], BF16)
    Rt = A([512], BF16)
    Vb = A([512], BF16)
    loT = A([2, 128], BF16)
    ART = A([4, 2, 128], BF16)
    BT = A([4, 128], BF16)
    KTt = A([4, 128], BF16)
    Nn = []
    for _ in range(2):
        Nn.append(A([8, 128], BF16))
        Nn[-1].at = ar.last_at
    Ll = []
    for _ in range(2):
        Ll.append(A([8, 128], BF16))
        Ll[-1].at = ar.last_at
    Lp2.append(A([1024], BF16, at=Nn[0].at, toks=Nn[0].toks))
    AT2.append(A([1024], BF16, at=Ll[0].at, toks=Ll[0].toks))
    MrbT = A([8, 128], BF16)
    LakT = A([8, 128], BF16)
    MrkT = A([8, 128], BF16)
    Yb = A([8, 128], BF16)
    Yb.at = ar.last_at
    RhT = A([8, 128], BF16, at=Ll[1].at, toks=Ll[1].toks)
    PhiT = A([8, 64], F32)
    Psig = A([8, 64], F32)
    gC = A([8], F32)
    Fs = A([8], F32)
    mix = A([1024], BF16, at=Nn[1].at, toks=Nn[1].toks)
    mixT = A([8, 128], BF16, at=Yb.at, toks=Yb.toks)
    xn2T = mixT
    rl = A([64], F32)
    rtmp = A([80], F32)
    print("mixer sbuf el", ar.off, "of", ar.n, flush=True)

    tokK, tokB = Tok(), Tok()
    v3 = lambda ap: ap.rearrange("p (h d) -> p h d", h=8)

    print("MARK setup end", P.nadd, flush=True)
    def xload(tg):
        dma("sp", xt[tg % 2].ap, x[tg * 128:(tg + 1) * 128, :], [], [xt[tg % 2]])

    def head_steps(tg):
        i = tg % NT
        xT_c = xnT[tg % 2]
        xT_p = xnT[(tg + 1) % 2]
        xtile = xt[tg % 2]

        def s0():
            act(xnb.ap, xtile.ap, AF.Square, [xtile], [xnb, st8], accum=st8[:, 0:1])

        def s1():
            ts("dve", st8[:, 1:2], st8[:, 0:1], 1.0 / 1024, 1e-6, ALU.mult, ALU.add, [st8], [st8])
            act(st8[:, 2:3], st8[:, 1:2], AF.Ln, [st8], [st8])
            act(st8[:, 3:4], st8[:, 2:3], AF.Exp, [st8], [st8], scale=-0.5)

        def s2():
            ts("dve", xnb.ap, xtile.ap, st8[:, 3:4], None, ALU.mult, None, [xtile, st8], [xnb])

        def s3():
            for c in range(8):
                tr(pT[:, c * 128:(c + 1) * 128], xnb[:, c * 128:(c + 1) * 128], identb, [xnb, cb], [pT])

        def s4():
            cp("act", xT_c[:, :, 1:129], pT.ap.rearrange("p (c t) -> p c t", c=8), [pT], [xT_c])
            if i == 0:
                P.add("pool", lambda e, t=xT_c: e.memset(t[:, :, 0:1], 0.0), [], [xT_c])
            else:
                cp("dve", xT_c[:, :, 0:1], xT_p[:, :, 128:129], [xT_p], [xT_c])
        return [s0, s1, s2, s3, s4]

    def head(tg):
        for f in head_steps(tg):
            f()

    def rw_inproj(tg):
        xT = xnT[tg % 2]
        rkv = [S_[0], S_[1], S_[2]]
        for q3 in range(3):
            pcur = pb[1][:, 0:512]
            pprv = pb[1][:, 512:1024]
            for c in range(8):
                mm(pcur, xT[:, c, 1:129], Wrw[:, c, q3 * 512:(q3 + 1) * 512], c == 0, c == 7, [Wrw, xT], [pb[1]])
            for c in range(8):
                mm(pprv, xT[:, c, 0:128], Wrw[:, c, q3 * 512:(q3 + 1) * 512], c == 0, c == 7, [Wrw, xT], [pb[1]])
            cp("act", S_[3].ap, pcur, [pb[1]], [S_[3]])
            tt("dve", S_[4].ap, pprv, S_[3].ap, ALU.subtract, [pb[1], S_[3]], [S_[4]])
            tt("dve", S_[4].ap, S_[4].ap, bct[:, BC_MU + q3 * 512:BC_MU + (q3 + 1) * 512], ALU.mult, [S_[4], bct], [S_[4]])
            tt("dve", rkv[q3].ap, S_[4].ap, S_[3].ap, ALU.add, [S_[4], S_[3]], [rkv[q3]])
        cp("dve", Vb.ap, S_[2].ap, [S_[2]], [Vb])

    xload(0)
    head(0)
    rw_inproj(0)
    for s in range(NSEQ):
        P.add("dve", lambda e: e.memset(Hst.ap, 0.0), [], [Hst])
        P.add("dve", lambda e: e.memset(Hb.ap, 0.0), [], [Hb])
        for i in range(NT):
            tg = s * NT + i
            xT_c = xnT[tg % 2]
            xtile = xt[tg % 2]
            row0 = tg * 128
            if tg + 1 < NSEQ * NT:
                xload(tg + 1)
            cur = lambda c: xT_c[:, c, 1:129]
            prv = lambda c: xT_c[:, c, 0:128]

            print("MARK tile", tg, "inproj", P.nadd, flush=True)
            r_, k_, v_ = S_[0], S_[1], S_[2]
            print("MARK lora", P.nadd, flush=True)
            for q2 in range(2):
                c0 = 1536 + q2 * 128
                for c in range(8):
                    mm(pc[:, q2 * 256:q2 * 256 + 128], Wrw[:, c, c0:c0 + 128], cur(c), c == 0, c == 7, [Wrw, xT_c], [pc])
                for c in range(8):
                    mm(pc[:, q2 * 256 + 128:q2 * 256 + 256], Wrw[:, c, c0:c0 + 128], prv(c), c == 0, c == 7, [Wrw, xT_c], [pc])
            for j in range(4):
                for c in range(8):
                    mm(pb[0][:, j * 128:(j + 1) * 128], Wsb[:, c, j * 128:(j + 1) * 128], cur(c), c == 0, c == 7, [Wsb, xT_c], [pb[0]])
            for j in range(4):
                for c in range(8):
                    mm(pb[0][:, 512 + j * 128:512 + (j + 1) * 128], Wsb[:, c, 512 + j * 128:512 + (j + 1) * 128], cur(c), c == 0, c == 7, [Wsb, xT_c], [pb[0]])
            lo = S_[3]
            stK = Tile(st8[:, 8:32], [tokK])
            stB = Tile(st8[:, 32:40], [tokB])
            for q2 in range(2):
                ts("dve", lo[:, q2 * 128:(q2 + 1) * 128], pc[:, q2 * 256:q2 * 256 + 128], omm[:, q2:q2 + 1], None, ALU.mult, None, [pc, omm], [lo])
                stt("dve", lo[:, q2 * 128:(q2 + 1) * 128], pc[:, q2 * 256 + 128:q2 * 256 + 256], pkt[:, PK_MUL + q2:PK_MUL + q2 + 1],
                    lo[:, q2 * 128:(q2 + 1) * 128], ALU.mult, ALU.add, [pc, pkt, lo], [lo])
            act(lo[0:64, 256:384], lo[0:64, 0:128], AF.Exp, [lo], [lo], scale=2.0)
            act(lo[:, 384:512], lo[:, 128:256], AF.Exp, [lo], [lo], scale=-1.0)
            cp("act", qT.ap, pb[0][:, 0:512].rearrange("p (j t) -> p j t", j=4), [pb[0]], [qT])
            cp("act", kT[:, :, i * 128:(i + 1) * 128], pb[0][:, 512:1024].rearrange("p (j t) -> p j t", j=4), [pb[0]], [kT])
            kkn = S_[8]
            tt("dve", kkn.ap, k_.ap, bct[:, BC_KK:BC_KK + 512], ALU.mult, [k_, bct], [kkn])
            tt("dve", S_[9].ap, kkn.ap, kkn.ap, ALU.mult, [kkn], [S_[9]])
            red("dve", stK[:, 0:8], v3(S_[9].ap), ALU.add, [S_[9]], [stK])
            ts("dve", stK[:, 0:8], stK[:, 0:8], 1e-24, None, ALU.max, None, [stK], [stK])
            ts("dve", lo[0:64, 256:384], lo[0:64, 256:384], 1.0, None, ALU.add, None, [lo], [lo])
            recip(lo[0:64, 256:384], lo[0:64, 256:384], [lo], [lo])
            ts("dve", loT[0:64, 0, :], lo[0:64, 256:384], -2.0, 1.0, ALU.mult, ALU.add, [lo], [loT])
            cp("dve", loT[64:128, 0, :], lo[64:128, 0:128], [lo], [loT])
            ts("dve", lo[:, 384:512], lo[:, 384:512], 1.0, None, ALU.add, None, [lo], [lo])
            recip(lo[:, 384:512], lo[:, 384:512], [lo], [lo])
            cp("dve", loT[:, 1, :], lo[:, 384:512], [lo], [loT])
            mm(pb[2][:, 0:512], loT[0:64, 0, :], lwb[0:64, 0:512], True, True, [loT, lwb], [pb[2]])
            mm(pb[2][:, 512:1024], loT[64:128, 0, :], lwb[64:128, 0:512], True, True, [loT, lwb], [pb[2]])
            mm(pc[:, 0:512], loT[:, 1, :], lwb[:, 512:1024], True, True, [loT, lwb], [pc])
            act(stK[:, 8:16], stK[:, 0:8], AF.Ln, [stK], [stK])
            act(stK[:, 16:24], stK[:, 8:16], AF.Exp, [stK], [stK], scale=-0.5)
            g_ = S_[7]
            cp("act", g_.ap, pc[:, 0:512], [pc], [g_])
            for c in range(8):
                mm(pc[:, 0:512], cur(c), Wsb[:, c, 1024:1536], c == 0, c == 7, [Wsb, xT_c], [pc])
            lg = S_[4]
            a_ = S_[6]
            tt("dve", lg.ap, pb[2][:, 0:512], bct[:, BC_W0:BC_W0 + 512], ALU.add, [pb[2], bct], [lg])
            tt("dve", a_.ap, pb[2][:, 512:1024], bct[:, BC_A0:BC_A0 + 512], ALU.add, [pb[2], bct], [a_])
            act(lg.ap, lg.ap, AF.Exp, [lg], [lg], scale=-1.0)
            act(a_.ap, a_.ap, AF.Exp, [a_], [a_], scale=-1.0)
            cp("act", vs[:, i, :], pc[:, 0:512], [pc], [vs])
            tt("dve", v3(kkn.ap), v3(kkn.ap), stK[:, 16:24].unsqueeze(2).to_broadcast([128, 8, 64]), ALU.mult, [kkn, stK], [kkn])
            ts("dve", lg.ap, lg.ap, 1.0, None, ALU.add, None, [lg], [lg])
            recip(lg.ap, lg.ap, [lg], [lg])
            ts("dve", lg.ap, lg.ap, -0.6065306597126334, None, ALU.mult, None, [lg], [lg])
            mm(pb[2][:, 0:512], mIN, lg.ap, True, True, [pkt, lg], [pb[2]])
            cl = S_[5]
            cp("act", cl.ap, pb[2][:, 0:512], [pb[2]], [cl])
            ts("dve", a_.ap, a_.ap, 1.0, None, ALU.add, None, [a_], [a_])
            recip(a_.ap, a_.ap, [a_], [a_])
            km = S_[9]
            ts("dve", km.ap, a_.ap, -1.0, None, ALU.add, None, [a_], [km])
            tt("dve", km.ap, km.ap, bct[:, BC_KA:BC_KA + 512], ALU.mult, [km, bct], [km])
            ts("dve", km.ap, km.ap, 1.0, None, ALU.add, None, [km], [km])
            tt("dve", km.ap, km.ap, k_.ap, ALU.mult, [km, k_], [km])
            dump(lg, 2048, row0)
            dump(a_, 2560, row0)
            dump(g_, 3072, row0)
            dump(cl, 3584, row0)
            dump(kkn, 4096, row0)
            dump(km, 4608, row0)
            dump(r_, 5632, row0)
            dump(k_, 6144, row0)
            dump(v_, 6656, row0)
            for h in range(8):
                mm(pc[0:64, h:h + 1], cl[:, h * 64:(h + 1) * 64], pkt[:, PK_ELAST:PK_ELAST + 1], True, True, [cl, pkt], [pc])
            eA, eN, eP = S_[10], S_[3], S_[4]
            tt("dve", eA.ap, cl.ap, lg.ap, ALU.subtract, [cl, lg], [eA])
            act(eA.ap, eA.ap, AF.Exp, [eA], [eA])
            act(eN.ap, cl.ap, AF.Exp, [cl], [eN], scale=-1.0)
            act(eP.ap, cl.ap, AF.Exp, [cl], [eP])
            act(gC[0:64, :], pc[0:64, 0:8], AF.Exp, [pc], [gC])
            stt("dve", Atc.ap, kkn.ap, -1.0, eA.ap, ALU.mult, ALU.mult, [kkn, eA], [Atc])
            cp("dve", At[:, :, 0:64], v3(Atc.ap), [Atc], [At])
            tt("dve", a_.ap, kkn.ap, a_.ap, ALU.mult, [kkn, a_], [a_])
            tt("dve", Bt.ap, a_.ap, eN.ap, ALU.mult, [a_, eN], [Bt])
            tt("dve", Kt.ap, km.ap, eN.ap, ALU.mult, [km, eN], [Kt])
            tt("dve", Rt.ap, r_.ap, eP.ap, ALU.mult, [r_, eP], [Rt])
            bon = S_[10]
            tt("dve", S_[3].ap, r_.ap, km.ap, ALU.mult, [r_, km], [S_[3]])
            tt("dve", S_[3].ap, S_[3].ap, bct[:, BC_RK:BC_RK + 512], ALU.mult, [S_[3], bct], [S_[3]])
            red("dve", stB[:, 0:8], v3(S_[3].ap), ALU.add, [S_[3]], [stB])
            tt("dve", v3(bon.ap), v3(v_.ap), stB[:, 0:8].unsqueeze(2).to_broadcast([128, 8, 64]), ALU.mult, [v_, stB], [bon])

            print("MARK fmajor", P.nadd, flush=True)
            for hp in range(4):
                tr(pT[:, hp * 128:(hp + 1) * 128], Atc[:, hp * 128:(hp + 1) * 128], identb, [Atc, cb], [pT])
                tr(pT[:, 512 + hp * 128:512 + (hp + 1) * 128], Rt[:, hp * 128:(hp + 1) * 128], identb, [Rt, cb], [pT])
            cp("act", ART[:, :, 0, :], pT[:, 0:512].rearrange("p (a t) -> p a t", a=4), [pT], [ART])
            cp("act", ART[:, :, 1, :], pT[:, 512:1024].rearrange("p (a t) -> p a t", a=4), [pT], [ART])
            for hp in range(4):
                tr(pT[:, hp * 128:(hp + 1) * 128], Bt[:, hp * 128:(hp + 1) * 128], identb, [Bt, cb], [pT])
                tr(pT[:, 512 + hp * 128:512 + (hp + 1) * 128], Kt[:, hp * 128:(hp + 1) * 128], identb, [Kt, cb], [pT])
            cp("act", BT.ap, pT[:, 0:512].rearrange("p (a t) -> p a t", a=4), [pT], [BT])
            cp("act", KTt.ap, pT[:, 512:1024].rearrange("p (a t) -> p a t", a=4), [pT], [KTt])

            N0, L0 = Nn[0], Ll[0]
            for hg in range(2):
                for hh in range(4):
                    h = hg * 4 + hh
                    hp, b0 = h // 2, (h % 2) * 64
                    par, q = hh % 2, hh // 2
                    rhs2 = ART[b0:b0 + 64, hp, :, :]
                    o1 = par * 512 + q * 256
                    o3 = par * 512 + q * 128
                    mm(pb[0][:, o1:o1 + 256], BT[b0:b0 + 64, hp, :], rhs2, True, True, [BT, ART], [pb[0]])
                    mm(pb[1][:, o1:o1 + 256], KTt[b0:b0 + 64, hp, :], rhs2, True, True, [KTt, ART], [pb[1]])
                    mm(pb[2][:, o3:o3 + 128], ART[b0:b0 + 64, hp, 0, :], BT[b0:b0 + 64, hp, :], True, True, [BT, ART], [pb[2]])
                g1 = pb[0].ap.rearrange("p (par q two t) -> p par q two t", par=2, q=2, two=2)
                g2 = pb[1].ap.rearrange("p (par q two t) -> p par q two t", par=2, q=2, two=2)
                g3 = pb[2].ap.rearrange("p (par x) -> p par x", par=2)
                bSU = mSU.unsqueeze(1).to_broadcast([128, 2, 128])
                bIN = mIN.unsqueeze(1).to_broadcast([128, 2, 128])
                bLO = mLO.unsqueeze(1).to_broadcast([128, 2, 128])
                hv = lambda T_: T_.ap.rearrange("p (g q par) t -> p g par q t", g=2, q=2, par=2)
                for par in range(2):
                    tt("dve", hv(N0)[:, hg, par], g1[:, par, :, 0, :], bSU, ALU.mult, [pb[0], pkt], [N0])
                    tt("dve", hv(MrbT)[:, hg, par], g1[:, par, :, 1, :], bIN, ALU.mult, [pb[0], pkt], [MrbT])
                    tt("dve", hv(LakT)[:, hg, par], g2[:, par, :, 0, :], bSU, ALU.mult, [pb[1], pkt], [LakT])
                    tt("dve", hv(MrkT)[:, hg, par], g2[:, par, :, 1, :], bIN, ALU.mult, [pb[1], pkt], [MrkT])
                    tt("dve", hv(L0)[:, hg, par], g3[:, par, 0:256].rearrange("p (q t) -> p q t", q=2), bLO, ALU.mult, [pb[2], pkt], [L0])
            for h in range(8):
                mm(pc[:, h * 64:(h + 1) * 64], LakT[:, h, :], Vb[:, h * 64:(h + 1) * 64], True, True, [LakT, Vb], [pc])
            cp("act", At[:, :, 64:128], pc.ap.rearrange("p (h d) -> p h d", h=8), [pc], [At])
            print("MARK neumann", P.nadd, flush=True)
            Ycur = At
            Ncur, Lcur = N0, L0
            for bk in range(2):
                mm(pb[2][:, bk * 512:(bk + 1) * 512], identb, At[:, 4 * bk:4 * bk + 4, :], True, False, [cb, At], [pb[2]])
            hst_ = head_steps(tg + 1) if tg + 1 < NSEQ * NT else []
            for lvl in range(7):
                for h in range(8):
                    mm(pb[2][:, h * 128:(h + 1) * 128], Ncur[:, h, :], Ycur[:, h, :], False, lvl == 6, [Ncur, Ycur], [pb[2]])
                if lvl < 6:
                    Nnx, Lnx = Nn[(lvl + 1) % 2], Ll[(lvl + 1) % 2]
                    for h in range(8):
                        mm(pb[0][:, h * 128:(h + 1) * 128], Lcur[:, h, :], Ncur[:, h, :], True, True, [Lcur, Ncur], [pb[0]])
                    if lvl < 5:
                        for h in range(8):
                            mm(pb[1][:, h * 128:(h + 1) * 128], Ncur[:, h, :], Lcur[:, h, :], True, True, [Lcur, Ncur], [pb[1]])
                    cp("act", Yb.ap, pb[2].ap.rearrange("p (h t) -> p h t", h=8), [pb[2]], [Yb])
                    cp("dve", Nnx.ap, pb[0].ap.rearrange("p (h t) -> p h t", h=8), [pb[0]], [Nnx])
                    if lvl < 5:
                        cp("dve", Lnx.ap, pb[1].ap.rearrange("p (h t) -> p h t", h=8), [pb[1]], [Lnx])
                    Ycur, Ncur, Lcur = Yb, Nnx, Lnx
                if lvl < len(hst_):
                    hst_[lvl]()
            cp("act", Yb.ap, pb[2].ap.rearrange("p (h t) -> p h t", h=8), [pb[2]], [Yb])
            AU = Yb
            print("MARK rhat", P.nadd, flush=True)
            for h in range(8):
                mm(pb[0][0:64, h * 128:(h + 1) * 128], AU[:, h, 0:64], MrbT[:, h, :], True, False, [AU, MrbT], [pb[0]])
                mm(pb[0][0:64, h * 128:(h + 1) * 128], Rt[:, h * 64:(h + 1) * 64], identb, False, True, [Rt, cb], [pb[0]])
            cp("act", RhT[0:64, :, :], pb[0][0:64, :].rearrange("p (h t) -> p h t", h=8), [pb[0]], [RhT])
            for h in range(8):
                mm(pb[1][0:64, h * 64:(h + 1) * 64], AU[:, h, 0:64], Bt[:, h * 64:(h + 1) * 64], True, True, [AU, Bt], [pb[1]])
                mm(pb[1][0:64, 512 + h * 64:512 + (h + 1) * 64], Bt[:, h * 64:(h + 1) * 64], AU[:, h, 64:128], True, False, [AU, Bt], [pb[1]])
                mm(pb[1][0:64, 512 + h * 64:512 + (h + 1) * 64], Kt[:, h * 64:(h + 1) * 64], Vb[:, h * 64:(h + 1) * 64], False, True, [Kt, Vb], [pb[1]])
            tt("dve", PhiT[0:64, :, :], pb[1][0:64, 0:512].rearrange("p (h d) -> p h d", h=8),
               pkt[0:64, PK_ID:PK_ID + 64].unsqueeze(1).to_broadcast([64, 8, 64]), ALU.add, [pb[1], pkt], [PhiT])
            tt("dve", Psig[0:64, :, :], pb[1][0:64, 512:1024].rearrange("p (h d) -> p h d", h=8),
               gC[0:64, :].unsqueeze(2).to_broadcast([64, 8, 64]), ALU.mult, [pb[1], gC], [Psig])
            for h in range(8):
                mm(pc[:, h * 64:(h + 1) * 64], RhT[0:64, h, :], Hb[0:64, h, :], True, False, [RhT, Hb], [pc])
                mm(pc[:, h * 64:(h + 1) * 64], MrbT[:, h, :], AU[:, h, 64:128], False, False, [MrbT, AU], [pc])
                mm(pc[:, h * 64:(h + 1) * 64], MrkT[:, h, :], Vb[:, h * 64:(h + 1) * 64], False, True, [MrkT, Vb], [pc])
            orw = S_[3]
            cp("act", orw.ap, pc[:, 0:512], [pc], [orw])
            dump(orw, 5120, row0)
            for h in range(8):
                mm(pb[0][0:64, h * 64:(h + 1) * 64], PhiT[0:64, h, :], Hst[0:64, h, :], True, True, [PhiT, Hst], [pb[0]])
            tt("dve", Hst[0:64, :, :], pb[0][0:64, 0:512].rearrange("p (h d) -> p h d", h=8),
               gC[0:64, :].unsqueeze(2).to_broadcast([64, 8, 64]), ALU.mult, [pb[0], gC], [Hst])
            tt("dve", Hst[0:64, :, :], Hst[0:64, :, :], Psig[0:64, :, :], ALU.add, [Hst, Psig], [Hst])
            cp("dve", Hb[0:64, :, :], Hst[0:64, :, :], [Hst], [Hb])
            KSKIP = _os.environ.get("KSKIP", "")
            blk = lambda h: (h % 2) * 4 + h // 2

            h8 = lambda ap: ap.rearrange("p (h t) -> p h t", h=8)

            def sb_z(kb):
                pz = pb[kb % 2]
                for h in range(8):
                    hp, b0 = h // 2, (h % 2) * 64
                    mm(pz[:, blk(h) * 128:(blk(h) + 1) * 128], kT[b0:b0 + 64, hp, kb * 128:(kb + 1) * 128], qT[b0:b0 + 64, hp, :], True, True, [kT, qT], [pz])

            def sb_el(kb):
                par_ = kb % 2
                E_, Lp, pz = E2[par_], Lp2[par_], pb[par_]
                act(E_.ap, pz.ap, AF.Exp, [pz], [E_])
                if kb == i:
                    tt("dve", h8(E_.ap), h8(E_.ap), mSU.unsqueeze(1).to_broadcast([128, 8, 128]), ALU.mult, [E_, pkt], [E_])
                act(Lp.ap, E_.ap, AF.Ln, [E_], [Lp], bias=1.0)

            def sb_cum(kb):
                Lp = Lp2[kb % 2]
                mm(pb[2][:, 0:512], negtri, Lp[:, 0:512], True, False, [cb, Lp], [pb[2]])
                mm(pb[2][:, 512:1024], negtri, Lp[:, 512:1024], True, False, [cb, Lp], [pb[2]])
                for h in range(8):
                    hp, b0 = h // 2, (h % 2) * 64
                    mm(pb[2][:, blk(h) * 128:(blk(h) + 1) * 128], kT[b0:b0 + 64, hp, kb * 128:(kb + 1) * 128], qT[b0:b0 + 64, hp, :], False, True, [kT, qT], [pb[2]])
                if kb != kbs[0]:
                    for h in range(8):
                        mm(pTf[:, h:h + 1], Lp[:, blk(h) * 128:(blk(h) + 1) * 128], negtri[:, 0:1], True, True, [Lp, cb], [pTf])

            def sb_w(kb):
                ATt = AT2[kb % 2]
                act(ATt.ap, pb[2].ap, AF.Exp, [pb[2]], [ATt])
                if kb == i:
                    tt("dve", h8(ATt.ap), h8(ATt.ap), mSU.unsqueeze(1).to_broadcast([128, 8, 128]), ALU.mult, [ATt, pkt], [ATt])
                if kb != kbs[0]:
                    act(Fs.ap, pTf[:, 0:8], AF.Exp, [pTf], [Fs])

            def sb_pv(kb):
                ATt = AT2[kb % 2]
                for h in range(8):
                    mm(pc[:, h * 64:(h + 1) * 64], ATt[:, blk(h) * 128:(blk(h) + 1) * 128], vs[:, kb, h * 64:(h + 1) * 64], True, True, [ATt, vs], [pc])
                if kb == kbs[0]:
                    cp("dve", acc.ap, pc[:, 0:512], [pc], [acc])
                else:
                    tt("dve", v3(acc.ap), v3(acc.ap), Fs.ap.unsqueeze(2).to_broadcast([128, 8, 64]), ALU.mult, [acc, Fs], [acc])
                    tt("dve", acc.ap, acc.ap, pc[:, 0:512], ALU.add, [acc, pc], [acc])

            kbs = [kb for kb in range(i + 1) if not ("s" in KSKIP and kb > 0)]
            sb_z(kbs[0])
            sb_el(kbs[0])
            if len(kbs) > 1:
                sb_z(kbs[1])
            red("dve", st8[:, 40:48], v3(orw.ap), ALU.add, [orw], [st8])
            ts("dve", st8[:, 40:48], st8[:, 40:48], 1.0 / 64, None, ALU.mult, None, [st8], [st8])
            tt("dve", v3(orw.ap), v3(orw.ap), st8[:, 40:48].unsqueeze(2).to_broadcast([128, 8, 64]), ALU.subtract, [orw, st8], [orw])
            tt("dve", S_[4].ap, orw.ap, orw.ap, ALU.mult, [orw], [S_[4]])
            red("dve", st8[:, 48:56], v3(S_[4].ap), ALU.add, [S_[4]], [st8])
            ts("dve", st8[:, 48:56], st8[:, 48:56], 1.0 / 64, 64e-5, ALU.mult, ALU.add, [st8], [st8])
            act(st8[:, 56:64], st8[:, 48:56], AF.Ln, [st8], [st8])
            act(st8[:, 48:56], st8[:, 56:64], AF.Exp, [st8], [st8], scale=-0.5)
            tt("dve", v3(orw.ap), v3(orw.ap), st8[:, 48:56].unsqueeze(2).to_broadcast([128, 8, 64]), ALU.mult, [orw, st8], [orw])
            tt("dve", orw.ap, orw.ap, bct[:, BC_LNW:BC_LNW + 512], ALU.mult, [orw, bct], [orw])
            tt("dve", orw.ap, orw.ap, bct[:, BC_LNB:BC_LNB + 512], ALU.add, [orw, bct], [orw])
            tt("dve", orw.ap, orw.ap, bon.ap, ALU.add, [orw, bon], [orw])
            tt("dve", mix[:, 512:1024], orw.ap, g_.ap, ALU.mult, [orw, g_], [mix])

            print("MARK sb", P.nadd, flush=True)
            for n_, kb in enumerate(kbs):
                sb_cum(kb)
                if n_ + 2 < len(kbs):
                    sb_z(kbs[n_ + 2])
                if n_ + 1 < len(kbs):
                    sb_el(kbs[n_ + 1])
                sb_w(kb)
                sb_pv(kb)
            tt("dve", S_[4].ap, acc.ap, acc.ap, ALU.mult, [acc], [S_[4]])
            red("dve", st8[:, 40:48], v3(S_[4].ap), ALU.add, [S_[4]], [st8])
            ts("dve", st8[:, 40:48], st8[:, 40:48], 1.0 / 64, 1e-6, ALU.mult, ALU.add, [st8], [st8])
            act(st8[:, 56:64], st8[:, 40:48], AF.Ln, [st8], [st8])
            act(st8[:, 40:48], st8[:, 56:64], AF.Exp, [st8], [st8], scale=-0.5)
            tt("dve", v3(mix[:, 0:512]), v3(acc.ap), st8[:, 40:48].unsqueeze(2).to_broadcast([128, 8, 64]), ALU.mult, [acc, st8], [mix])

            print("MARK outproj", P.nadd, flush=True)
            for c in range(8):
                tr(pT[:, c * 128:(c + 1) * 128], mix[:, c * 128:(c + 1) * 128], identb, [mix, cb], [pT])
            cp("act", mixT.ap, pT.ap.rearrange("p (c t) -> p c t", c=8), [pT], [mixT])
            for half in range(2):
                for c in range(8):
                    mm(pb[0][:, half * 512:(half + 1) * 512], mixT[:, c, :], Wout[:, c, half * 512:(half + 1) * 512], c == 0, c == 7, [mixT, Wout], [pb[0]])
            ht = xtile
            tt("dve", ht.ap, pb[0].ap, xtile.ap, ALU.add, [pb[0], xtile], [ht])
            dma("sp", hbuf[row0:row0 + 128, :], ht.ap, [ht], [])
            if dbg:
                dma("sp", dbg_t[row0:row0 + 128, 0:1024], ht.ap, [ht], [], final=True)
            act(xn2.ap, ht.ap, AF.Square, [ht], [xn2, st8], accum=st8[:, 4:5])
            ts("dve", st8[:, 5:6], st8[:, 4:5], 1.0 / 1024, 1e-6, ALU.mult, ALU.add, [st8], [st8])
            act(st8[:, 6:7], st8[:, 5:6], AF.Ln, [st8], [st8])
            act(st8[:, 7:8], st8[:, 6:7], AF.Exp, [st8], [st8], scale=-0.5)
            stt("dve", xn2.ap, ht.ap, st8[:, 7:8], bct[:, BC_GFFN:BC_GFFN + 1024], ALU.mult, ALU.mult, [ht, st8, bct], [xn2])
            if tg + 1 < NSEQ * NT:
                rw_inproj(tg + 1)
            for c in range(8):
                tr(pT[:, c * 128:(c + 1) * 128], xn2[:, c * 128:(c + 1) * 128], identb, [xn2, cb], [pT])
            cp("act", xn2T.ap, pT.ap.rearrange("p (c t) -> p c t", c=8), [pT], [xn2T])
            dma("sp", x2d[:, :, row0:row0 + 128], xn2T.ap, [xn2T], [])
            for c in range(8):
                mm(pc[:, 0:36], xn2T[:, c, :], Wr[:, c, :], c == 0, c == 7, [xn2T, Wr], [pc])
            tt("dve", rl[:, 0:36], pc[:, 0:36], bct[:, BC_RB:BC_RB + 36], ALU.add, [pc, bct], [rl])
            red("dve", st8[:, 40:41], rl[:, 0:4], ALU.max, [rl], [st8])
            ts("dve", rtmp[:, 0:4], rl[:, 0:4], st8[:, 40:41], None, ALU.is_equal, None, [rl, st8], [rtmp])
            ts("dve", rtmp[:, 4:8], rl[:, 0:4], st8[:, 40:41], None, ALU.subtract, None, [rl, st8], [rtmp])
            act(rtmp[:, 4:8], rtmp[:, 4:8], AF.Exp, [rtmp], [rtmp, st8], accum=st8[:, 41:42])
            P.add("dve", lambda e: e.reciprocal(out=st8[:, 42:43], in_=st8[:, 41:42]), [st8], [st8])
            tt("dve", rtmp[:, 8:40].rearrange("p (g j) -> p g j", g=4), rl[:, 4:36].rearrange("p (g j) -> p g j", g=4),
               rtmp[:, 0:4].unsqueeze(2).to_broadcast([128, 4, 8]), ALU.mult, [rl, rtmp], [rtmp])
            red("dve", rtmp[:, 40:48], rtmp[:, 8:40].rearrange("p (g j) -> p j g", g=4), ALU.add, [rtmp], [rtmp])
            red("dve", st8[:, 43:44], rtmp[:, 40:48], ALU.max, [rtmp], [st8])
            ts("dve", rtmp[:, 48:56], rtmp[:, 40:48], st8[:, 43:44], None, ALU.is_equal, None, [rtmp, st8], [rtmp])
            stt("dve", rtmp[:, 56:64], rtmp[:, 48:56], -1e30, rtmp[:, 40:48], ALU.mult, ALU.add, [rtmp], [rtmp])
            red("dve", st8[:, 44:45], rtmp[:, 56:64], ALU.max, [rtmp], [st8])
            ts("dve", rtmp[:, 64:72], rtmp[:, 56:64], st8[:, 44:45], None, ALU.is_equal, None, [rtmp, st8], [rtmp])
            tt("dve", st8[:, 45:46], st8[:, 44:45], st8[:, 43:44], ALU.subtract, [st8], [st8])
            act(st8[:, 46:47], st8[:, 45:46], AF.Exp, [st8], [st8])
            ts("dve", st8[:, 47:48], st8[:, 46:47], 1.0, None, ALU.add, None, [st8], [st8])
            P.add("dve", lambda e: e.reciprocal(out=st8[:, 47:48], in_=st8[:, 47:48]), [st8], [st8])
            tt("dve", st8[:, 47:48], st8[:, 47:48], st8[:, 42:43], ALU.mult, [st8], [st8])
            tt("dve", st8[:, 46:47], st8[:, 46:47], st8[:, 47:48], ALU.mult, [st8], [st8])
            ts("dve", rtmp[:, 72:80], rtmp[:, 48:56], st8[:, 47:48], None, ALU.mult, None, [rtmp, st8], [rtmp])
            stt("dve", rtmp[:, 72:80], rtmp[:, 64:72], st8[:, 46:47], rtmp[:, 72:80], ALU.mult, ALU.add, [rtmp, st8], [rtmp])
            tt("dve", call[:, tg, :].rearrange("p (g j) -> p g j", g=4), rtmp[:, 0:4].unsqueeze(2).to_broadcast([128, 4, 8]),
               rtmp[:, 72:80].unsqueeze(1).to_broadcast([128, 4, 8]), ALU.mult, [rtmp], [call])
            if dbg:
                cp("dve", E_.ap, mix.ap, [mix], [E_])
                dma("sp", dbg_t[row0:row0 + 128, 1024:2048], E_.ap, [E_], [], final=True)

    print("MARK moe", P.nadd, flush=True)
    if _os.environ.get("KNOMOE"):
        P.stopped = True
    P.barrier()
    ar.off = mark_moe
    TB = min(2048, NTOK)
    NPASS = NTOK // TB
    TPB = TB // 128
    gfin = A([1024], F32)
    dma("sp", gfin.ap, bc[:, BC_GFIN:BC_GFIN + 1024], [], [gfin])
    x2 = A([8, TB], BF16)
    yh = []
    yacc = []
    for _t in range(TPB):
        h0_ = A([512], F32)
        at0 = ar.last_at
        h1_ = A([512], F32)
        yh.append((h0_, h1_))
        yacc.append(A([1024], F32, at=at0, toks=h0_.toks + h1_.toks))
    NWB = 2
    mstg = [A([2048], F32) for _ in range(4)]
    mi = 0
    Wg_ = [A([8, 256], BF16) for _ in range(NWB)]
    Wu_ = [A([8, 256], BF16) for _ in range(NWB)]
    Wd_ = [A([2, 1024], BF16) for _ in range(NWB)]
    hT = [A([2, TB], BF16) for _ in range(2)]
    sgt = [A([512], F32) for _ in range(2)]
    ysct = [A([512], F32) for _ in range(2)]
    st9 = A([8], F32)
    junk2 = A([1024], F32)
    ot = [A([1024], F32) for _ in range(2)]
    wi = 0
    for ps_ in range(NPASS):
        tok0 = ps_ * TB
        dma("sp", x2.ap, x2d[:, :, tok0:tok0 + TB], [], [x2])
        for t in range(TPB):
            dma("sp", yacc[t].ap, hbuf[tok0 + t * 128:tok0 + (t + 1) * 128, :], [], [yacc[t]])
        mi_ = [mi]
        k2_ = [0]
        yi_ = [0]
        tokY0, tokY1 = Tok(), Tok()

        def moe_load(ex):
            wb = ex % NWB
            for (dst, src, na_) in ((Wg_[wb], wg[ex], 8), (Wu_[wb], wu[ex], 8), (Wd_[wb], wd[ex], 2)):
                sgm = mstg[mi_[0] % len(mstg)]
                mi_[0] += 1
                sv = sgm.ap.rearrange("p (a b) -> p a b", a=na_)
                dma("sp", sv, src.rearrange("(c p) n -> p c n", p=128), [], [sgm])
                cp("pool", dst.ap, sv, [sgm], [dst])

        def moe_gu(ex):
            wb = ex % NWB
            hTe = hT[ex % 2]
            NB = min(512, TB)
            for hc in range(2):
                for tb in range(TB // NB):
                    pgu = pb[k2_[0] % 2]
                    sg_ = sgt[k2_[0] % 2]
                    k2_[0] += 1
                    for c in range(8):
                        mm(pgu[:, 0:NB], Wg_[wb][:, c, hc * 128:(hc + 1) * 128], x2[:, c, tb * NB:(tb + 1) * NB], c == 0, c == 7, [Wg_[wb], x2], [pgu])
                    for c in range(8):
                        mm(pgu[:, 512:512 + NB], Wu_[wb][:, c, hc * 128:(hc + 1) * 128], x2[:, c, tb * NB:(tb + 1) * NB], c == 0, c == 7, [Wu_[wb], x2], [pgu])
                    act(sg_[:, 0:NB], pgu[:, 0:NB], AF.Silu, [pgu], [sg_])
                    tt("dve", hTe[:, hc, tb * NB:(tb + 1) * NB], sg_[:, 0:NB], pgu[:, 512:512 + NB], ALU.mult, [sg_, pgu], [hTe])

        yring = [(pb[2][:, 0:512], Tile(pb[2][:, 0:512], [tokY0])), (pb[2][:, 512:1024], Tile(pb[2][:, 512:1024], [tokY1])),
                 (pc[:, 0:512], pc), (pTf[:, 0:512], pTf)]

        def moe_down(ex):
            wb = ex % NWB
            hTe = hT[ex % 2]
            for t in range(TPB):
                tgl = tok0 // 128 + t
                for half in range(2):
                    yp, ytok = yring[yi_[0] % 4]
                    yi_[0] += 1
                    for hc in range(2):
                        mm(yp, hTe[:, hc, t * 128:(t + 1) * 128], Wd_[wb][:, hc, half * 512:(half + 1) * 512],
                           hc == 0, hc == 1, [hTe, Wd_[wb]], [ytok])
                    stt("dve", yh[t][half].ap, yp, call[:, tgl, ex:ex + 1], yh[t][half].ap, ALU.mult, ALU.add,
                        [ytok, call, yh[t][half]], [yh[t][half]])

        moe_load(0)
        moe_load(1)
        moe_gu(0)
        for ex in range(32):
            if ex + 1 < 32:
                moe_gu(ex + 1)
            moe_down(ex)
            if ex + 2 < 32:
                moe_load(ex + 2)
        mi = mi_[0]
        for t in range(TPB):
            o_ = ot[t % 2]
            act(junk2.ap, yacc[t].ap, AF.Square, [yacc[t]], [junk2, st9], accum=st9[:, 0:1])
            ts("dve", st9[:, 1:2], st9[:, 0:1], 1.0 / 1024, 1e-6, ALU.mult, ALU.add, [st9], [st9])
            act(st9[:, 2:3], st9[:, 1:2], AF.Ln, [st9], [st9])
            act(st9[:, 3:4], st9[:, 2:3], AF.Exp, [st9], [st9], scale=-0.5)
            stt("dve", o_.ap, yacc[t].ap, st9[:, 3:4], gfin.ap, ALU.mult, ALU.mult, [yacc[t], st9, gfin], [o_])
            dma("sp", out[tok0 + t * 128:tok0 + (t + 1) * 128, :], o_.ap, [o_], [], final=True)
    stats = P.emit()
    print("ops", stats, "sbuf hi bytes", ar.hi * 2, flush=True)
    return nc


_CACHE = {}


def _pack(inp):
    f = np.float32
    g = lambda k: np.asarray(inp[k], dtype=f)
    pk = np.zeros((128, NPK), f)
    pk[:, PK_GMIX:PK_GMIX + 8] = g("norm_mix_g")[0].reshape(8, 128).T
    pk[:, PK_SBG:PK_SBG + 4] = g("sb_out_g")[0].reshape(4, 128).T
    mu = g("shift_mu")[0]
    pk[:, PK_MUL] = mu[1536:1664]
    pk[:, PK_MUL + 1] = mu[1664:1792]
    pk[127, PK_ELAST] = 1.0
    idx = np.arange(128)
    pk[:, PK_ID:PK_ID + 128] = np.eye(128, dtype=f)
    pk[:, PK_SU:PK_SU + 128] = (idx[:, None] < idx[None, :]).astype(f)
    pk[:, PK_IN:PK_IN + 128] = (idx[:, None] <= idx[None, :]).astype(f)
    pk[:, PK_LO:PK_LO + 128] = (idx[None, :] < idx[:, None]).astype(f)
    pk[:, PK_NT:PK_NT + 128] = -(idx[:, None] >= idx[None, :]).astype(f)
    row = np.zeros((NBC,), f)
    row[BC_MU:BC_MU + 1536] = mu[0:1536]
    row[BC_W0:BC_W0 + 512] = g("rw_w0")[0]
    row[BC_A0:BC_A0 + 512] = g("rw_a0")[0]
    row[BC_KK:BC_KK + 512] = g("rw_k_k")[0]
    row[BC_KA:BC_KA + 512] = g("rw_k_a")[0]
    row[BC_RK:BC_RK + 512] = g("rw_r_k")[0].reshape(512)
    row[BC_LNW:BC_LNW + 512] = g("rw_ln_w")[0]
    row[BC_LNB:BC_LNB + 512] = g("rw_ln_b")[0]
    row[BC_GFFN:BC_GFFN + 1024] = g("norm_ffn_g")[0]
    row[BC_RB:BC_RB + 4] = g("router_grp_b")[0]
    row[BC_RB + 4:BC_RB + 36] = g("router_exp_b")[0]
    row[BC_GFIN:BC_GFIN + 1024] = g("final_norm_g")
    bcm = np.ascontiguousarray(np.broadcast_to(row[None, :], (128, NBC)))
    lwm = np.concatenate([np.concatenate([g("rw_w2")[0], g("rw_a2")[0]], axis=0), g("rw_g2")[0]], axis=1)
    wrm = np.concatenate([g("router_grp_w")[0], g("router_exp_w")[0]], axis=1)
    return dict(w_in=g("w_in")[0], w_out=g("w_out")[0], wr=np.ascontiguousarray(wrm), lw=np.ascontiguousarray(lwm),
                wg=g("exp_w_gate")[0], wu=g("exp_w_up")[0], wd=g("exp_w_down")[0], pk=pk, bc=bcm)


def run(inputs, ncores, dbg=False):
    x = np.asarray(inputs["x"], dtype=np.float32)
    B, S, D = x.shape
    nseq = B // ncores
    key = (S, nseq, dbg)
    if key not in _CACHE:
        _CACHE[key] = build(S, nseq, dbg)
    nc = _CACHE[key]
    shared = _pack(inputs)
    in_maps = []
    for c in range(ncores):
        m = dict(shared)
        m["x"] = np.ascontiguousarray(x[c * nseq:(c + 1) * nseq].reshape(nseq * S, D))
        in_maps.append(m)
    res = run_bass_kernel_spmd(nc, in_maps, core_ids=list(range(ncores)))
    out = np.stack([r["out"].reshape(nseq, S, D) for r in res.results], axis=0).reshape(B, S, D)
    if dbg:
        return out, [r["dbg"] for r in res.results]
    return out


def kernel(**inputs):
    return run(inputs, 8).astype(np.float32)
```

```python
import numpy as np
import ml_dtypes
import concourse.bass as bass
import concourse.mybir as mybir
from concourse.bass_utils import run_bass_kernel_spmd

F32 = mybir.dt.float32
BF16 = mybir.dt.bfloat16
AF = mybir.ActivationFunctionType
ALU = mybir.AluOpType
AX = mybir.AxisListType

PK_GMIX, PK_SBG, PK_MUL, PK_ELAST, PK_ID, PK_SU, PK_IN, PK_LO, PK_NT, NPK = 0, 8, 12, 14, 16, 144, 272, 400, 528, 656
BC_MU, BC_W0, BC_A0, BC_KK, BC_KA, BC_RK, BC_LNW, BC_LNB, BC_GFFN, BC_RB, BC_GFIN, NBC = (
    0, 1536, 2048, 2560, 3072, 3584, 4096, 4608, 5120, 6144, 6192, 7216)
BC_MIX = 6192


import os as _osmod
STRICT = bool(_osmod.environ.get("KSTRICT"))


class Tok:
    __slots__ = ("last_w", "readers")

    def __init__(self):
        self.last_w = None
        self.readers = []


class Tile:
    def __init__(self, ap, toks=None):
        self.ap = ap
        self.toks = toks if toks is not None else [Tok()]

    def __getitem__(self, k):
        return self.ap[k]


class Prog:
    ENGS = ("pe", "act", "dve", "pool", "sp")
    NDMA = 40

    def __init__(self, nc):
        self.nc = nc
        self.ops = {e: [] for e in self.ENGS}
        self.waited = {e: {} for e in self.ENGS}
        self.ndma = 0
        self.dma_last = [None] * self.NDMA
        self.final_dma = []
        self.last_compute = {e: None for e in self.ENGS}

    def _need(self, eng, ref, waits):
        kind, key, val = ref
        w = self.waited[eng]
        k = (kind, key)
        if w.get(k, -1) >= val:
            return
        w[k] = val
        waits.append(ref)
        if kind == "eng":
            self.ops[key][val]["signal"] = True

    def add(self, eng, fn, reads=(), writes=(), dma=False, final=False):
        import os
        self.nadd = getattr(self, "nadd", 0) + 1
        if self.nadd > int(os.environ.get("KSTOP", "100000000")) or getattr(self, "stopped", False):
            return None
        reads = [k for t in reads for k in t.toks]
        writes = [k for t in writes for k in t.toks]
        deps = []
        for t in reads:
            if t.last_w is not None:
                deps.append((t.last_w, True))
        for t in writes:
            if t.last_w is not None:
                deps.append((t.last_w, False))
            for r in t.readers:
                deps.append((r, False))
        waits = []
        for ref, raw in deps:
            kind, key, val = ref
            if kind == "eng" and key == eng and not dma:
                if eng == "pe" or (not raw and not STRICT):
                    continue
            self._need(eng, ref, waits)
        idx = len(self.ops[eng])
        op = {"fn": fn, "waits": waits, "signal": False, "dma": None}
        if dma:
            slot = self.ndma % self.NDMA
            val = 16 * (self.ndma // self.NDMA + 1)
            self.ndma += 1
            if self.dma_last[slot] is not None:
                self._need(eng, self.dma_last[slot], waits)
            myref = ("dma", slot, val)
            self.dma_last[slot] = myref
            op["dma"] = (slot, val)
            if final:
                self.final_dma.append(myref)
        else:
            myref = ("eng", eng, idx)
            self.last_compute[eng] = myref
        self.ops[eng].append(op)
        for t in reads:
            t.readers.append(myref)
        for t in writes:
            t.last_w = myref
            t.readers = []
        return myref

    def barrier(self):
        refs = [r for r in self.last_compute.values() if r is not None]
        refs += [r for r in self.dma_last if r is not None]
        for e in self.ENGS:
            waits = []
            for ref in refs:
                if ref[0] == "eng" and ref[1] == e:
                    continue
                self._need(e, ref, waits)
            if waits:
                self.ops[e].append({"fn": None, "waits": waits, "signal": False, "dma": None})

    def emit(self):
        nc = self.nc
        fw = []
        for ref in self.final_dma:
            self._need("sp", ref, fw)
        if fw:
            self.ops["sp"].append({"fn": None, "waits": fw, "signal": False, "dma": None})
        ordinal = {}
        for e in self.ENGS:
            c = 0
            for i, op in enumerate(self.ops[e]):
                if op["signal"]:
                    c += 1
                    ordinal[(e, i)] = c
            print("SEMMAX", e, c, flush=True)
        from contextlib import ExitStack
        with ExitStack() as st:
            esem = {e: st.enter_context(nc.semaphore(f"s_{e}")) for e in self.ENGS}
            dsem = [st.enter_context(nc.semaphore(f"d_{i}")) for i in range(self.NDMA)]
            block = st.enter_context(nc.Block())

            def run(e, engobj):
                for i, op in enumerate(self.ops[e]):
                    for kind, key, val in op["waits"]:
                        if kind == "eng":
                            engobj.wait_ge(esem[key], ordinal[(key, val)])
                        else:
                            engobj.wait_ge(dsem[key], val)
                    if op["fn"] is None:
                        continue
                    ins = op["fn"](engobj)
                    if op["dma"] is not None:
                        ins.then_inc(dsem[op["dma"][0]], 16)
                    elif op["signal"]:
                        ins.then_inc(esem[e], 1)

            block.tensor(lambda eng: run("pe", eng))
            block.scalar(lambda eng: run("act", eng))
            block.vector(lambda eng: run("dve", eng))
            block.gpsimd(lambda eng: run("pool", eng))
            block.sync(lambda eng: run("sp", eng))
        return {e: len(self.ops[e]) for e in self.ENGS}


class Arena:
    def __init__(self, nc, nbytes):
        self.n = nbytes // 2
        self.t = nc.alloc_sbuf_tensor("arena", [128, self.n], BF16)
        self.off = 0
        self.hi = 0

    def alloc(self, shape, dt, at=None, toks=None):
        n = int(np.prod(shape))
        el = n * (2 if dt == F32 else 1)
        el = (el + 15) // 16 * 16
        if at is None:
            assert self.off + el <= self.n, f"SBUF arena overflow {self.off + el} > {self.n}"
            at = self.off
            self.off += el
            self.hi = max(self.hi, self.off)
        ap = self.t[:, at:at + el]
        self.last_at = at
        if dt == F32:
            ap = ap.bitcast(F32)
        ap = ap[:, 0:n]
        if len(shape) == 2:
            ap = ap.rearrange("p (a b) -> p a b", a=shape[0], b=shape[1])
        elif len(shape) == 3:
            ap = ap.rearrange("p (a b c) -> p a b c", a=shape[0], b=shape[1], c=shape[2])
        return Tile(ap, toks)


def build(S, NSEQ, dbg=False):
    import os as _os
    NT = S // 128
    NTOK = S * NSEQ
    NTT = NTOK // 128
    nc = bass.Bass("TRN2", target_bir_lowering=False)
    din = lambda name, shape, dt=F32: nc.dram_tensor(name, shape, dt, kind="ExternalInput").ap()
    x = din("x", [NTOK, 1024])
    w_in = din("w_in", [1024, 3328])
    w_out = din("w_out", [1024, 1024])
    wr = din("wr", [1024, 36])
    lw = din("lw", [128, 1024])
    wg = din("wg", [32, 1024, 256])
    wu = din("wu", [32, 1024, 256])
    wd = din("wd", [32, 256, 1024])
    pk = din("pk", [128, NPK])
    bc = din("bc", [128, NBC])
    out = nc.dram_tensor("out", [NTOK, 1024], F32, kind="ExternalOutput").ap()
    hbuf = nc.dram_tensor("hbuf", [NTOK, 1024], F32).ap()
    x2d = nc.dram_tensor("x2d", [128, 8, NTOK], BF16).ap()
    dbg_t = nc.dram_tensor("dbg", [NTOK, 8192], F32, kind="ExternalOutput").ap() if dbg else None

    P = Prog(nc)
    ar = Arena(nc, 212800)
    A = ar.alloc

    pb = [Tile(nc.alloc_psum_tensor(f"pb{i}", [128, 1024], F32)[:]) for i in range(3)]
    pc = Tile(nc.alloc_psum_tensor("pc", [128, 512], F32)[:])
    pT = Tile(nc.alloc_psum_tensor("pT", [128, 1024], BF16)[:])

    pTf = Tile(pT.ap.bitcast(F32), pT.toks)

    def mm(o, lhsT, rhs, start, stop, r, w):
        P.add("pe", lambda e: e.matmul(o, lhsT=lhsT, rhs=rhs, start=start, stop=stop, skip_group_check=True), r, w)

    def tr(o, in_, ident, r, w):
        P.add("pe", lambda e: e.transpose(out=o, in_=in_, identity=ident), r, w)

    def tt(eng, o, a, b, op, r, w):
        P.add(eng, lambda e: e.tensor_tensor(out=o, in0=a, in1=b, op=op), r, w)

    def ts(eng, o, a, s1, s2, op0, op1, r, w):
        if s2 is None:
            P.add(eng, lambda e: e.tensor_scalar(out=o, in0=a, scalar1=s1, scalar2=None, op0=op0), r, w)
        else:
            P.add(eng, lambda e: e.tensor_scalar(out=o, in0=a, scalar1=s1, scalar2=s2, op0=op0, op1=op1), r, w)

    def stt(eng, o, a, s, b, op0, op1, r, w):
        P.add(eng, lambda e: e.scalar_tensor_tensor(out=o, in0=a, scalar=s, in1=b, op0=op0, op1=op1), r, w)

    def act(o, in_, func, r, w, bias=0.0, scale=1.0, accum=None):
        if accum is None:
            P.add("act", lambda e: e.activation(out=o, in_=in_, func=func, bias=bias, scale=scale), r, w)
        else:
            P.add("act", lambda e: e.activation(out=o, in_=in_, func=func, bias=bias, scale=scale, accum_out=accum), r, w)

    def cp(eng, o, in_, r, w):
        if eng == "act":
            P.add("act", lambda e: e.activation(out=o, in_=in_, func=AF.Copy), r, w)
        else:
            P.add(eng, lambda e: e.tensor_copy(out=o, in_=in_), r, w)

    def recip(o, in_, r, w):
        P.add("dve", lambda e: e.reciprocal(out=o, in_=in_), r, w)

    def red(eng, o, in_, op, r, w):
        P.add(eng, lambda e: e.tensor_reduce(out=o, in_=in_, axis=AX.X, op=op), r, w)

    def dma(eng, o, in_, r, w, final=False):
        P.add(eng, lambda e: e.dma_start(out=o, in_=in_), r, w, dma=True, final=final)

    def dump(tile, col, row0, n=512):
        if dbg:
            dma("sp", dbg_t[row0:row0 + 128, col:col + n], tile.ap, [tile], [], final=True)

    def rsqrt_small(o, in_, r, w, tmp):
        act(tmp, in_, AF.Ln, r, [tmp_tok(tmp)])
        act(o, tmp, AF.Exp, [tmp_tok(tmp)], w, scale=-0.5)

    def tmp_tok(t):
        return t

    call = A([NTT, 32], F32)
    pkt = A([NPK], F32)
    mark_moe = ar.off
    bct = A([BC_MIX], F32)
    dma("sp", pkt.ap, pk, [], [pkt])
    dma("sp", bct.ap, bc[:, 0:BC_MIX], [], [bct])
    ident_f = pkt[:, PK_ID:PK_ID + 128]
    mSU = pkt[:, PK_SU:PK_SU + 128]
    mIN = pkt[:, PK_IN:PK_IN + 128]
    mLO = pkt[:, PK_LO:PK_LO + 128]
    cb = A([4, 128], BF16)
    cp("dve", cb[:, 0, :], ident_f, [pkt], [cb])
    cp("dve", cb[:, 1, :], pkt[:, PK_NT:PK_NT + 128], [pkt], [cb])
    identb = cb[:, 0, :]
    negtri = cb[:, 1, :]
    omm = A([2], F32)
    ts("dve", omm.ap, pkt[:, PK_MUL:PK_MUL + 2], -1.0, 1.0, ALU.mult, ALU.add, [pkt], [omm])

    Wsb = A([8, 1536], BF16)
    Wrw = A([8, 1792], BF16)
    Wout = A([8, 1024], BF16)
    Wr = A([8, 36], BF16)
    lwb = A([1024], BF16)
    kT = A([4, S], BF16)
    vs = A([NT, 512], BF16)
    Hst = A([8, 64], F32)
    Hb = A([8, 64], BF16)
    mark = ar.off

    stg = [A([8, 256], F32) for _ in range(2)]
    w_in_v = w_in.rearrange("(c p) n -> p c n", p=128)
    w_out_v = w_out.rearrange("(c p) n -> p c n", p=128)
    si = 0
    for c0 in range(0, 3328, 256):
        cw = 256
        sg = stg[si % 2]
        si += 1
        dma("sp", sg[:, :, 0:cw], w_in_v[:, :, c0:c0 + cw], [], [sg])
        for c in range(8):
            eng = "dve"
            if c0 < 1536:
                dst = Wsb[:, c, c0:c0 + cw]
                dt_ = Wsb
            else:
                dst = Wrw[:, c, c0 - 1536:c0 - 1536 + cw]
                dt_ = Wrw
            if c0 < 512:
                ts(eng, dst, sg[:, c, 0:cw], pkt[:, PK_GMIX + c:PK_GMIX + c + 1], 0.125, ALU.mult, ALU.mult, [sg, pkt], [dt_])
            else:
                ts(eng, dst, sg[:, c, 0:cw], pkt[:, PK_GMIX + c:PK_GMIX + c + 1], None, ALU.mult, None, [sg, pkt], [dt_])
    for c0 in range(0, 1024, 256):
        sg = stg[si % 2]
        si += 1
        dma("sp", sg.ap, w_out_v[:, :, c0:c0 + 256], [], [sg])
        for c in range(8):
            eng = "dve"
            if c < 4:
                ts(eng, Wout[:, c, c0:c0 + 256], sg[:, c, :], pkt[:, PK_SBG + c:PK_SBG + c + 1], None, ALU.mult, None, [sg, pkt], [Wout])
            else:
                cp(eng, Wout[:, c, c0:c0 + 256], sg[:, c, :], [sg], [Wout])
    sg = stg[si % 2]
    si += 1
    dma("sp", sg[:, :, 0:36], wr.rearrange("(c p) n -> p c n", p=128), [], [sg])
    cp("dve", Wr.ap, sg[:, :, 0:36], [sg], [Wr])
    sg = stg[si % 2]
    si += 1
    dma("sp", sg[:, 0:4, :], lw.rearrange("p (a b) -> p a b", a=4), [], [sg])
    cp("dve", lwb.ap.rearrange("p (a b) -> p a b", a=4), sg[:, 0:4, :], [sg], [lwb])
    P.barrier()
    ar.off = mark

    xt = [A([1024], F32) for _ in range(2)]
    st8 = A([64], F32)
    xnb = A([1024], BF16)
    xnT = [A([8, 129], BF16) for _ in range(2)]
    qT = A([4, 128], BF16)
    S_ = []
    for _ in range(11):
        S_.append(A([512], F32))
        S_[-1].at = ar.last_at
    al2 = lambda sl, shape, dt: A(shape, dt, at=S_[sl[0]].at, toks=[k for j in sl for k in S_[j].toks])
    E2 = [al2([5, 6], [1024], F32), al2([3, 4], [1024], F32)]
    X2 = [al2([7, 8], [1024], F32), al2([9, 10], [1024], F32)]
    Lp2 = [al2([0], [1024], BF16)]
    AT2 = [al2([1], [1024], BF16)]
    acc = al2([2], [512], F32)
    xn2 = al2([7], [1024], BF16)
    E_ = E2[0]
    Atc = A([512], BF16)
    At = A([8, 128], BF16)
    Bt = A([512], BF16)
    Kt = A([512], BF16)
    Rt = A([512], BF16)
    Vb = A([512], BF16)
    loT = A([2, 128], BF16)
    ART = A([4, 2, 128], BF16)
    BT = A([4, 128], BF16)
    KTt = A([4, 128], BF16)
    Nn = []
    for _ in range(2):
        Nn.append(A([8, 128], BF16))
        Nn[-1].at = ar.last_at
    Ll = []
    for _ in range(2):
        Ll.append(A([8, 128], BF16))
        Ll[-1].at = ar.last_at
    Lp2.append(A([1024], BF16, at=Nn[0].at, toks=Nn[0].toks))
    AT2.append(A([1024], BF16, at=Ll[0].at, toks=Ll[0].toks))
    MrbT = A([8, 128], BF16)
    LakT = A([8, 128], BF16)
    MrkT = A([8, 128], BF16)
    Yb = A([8, 128], BF16)
    Yb.at = ar.last_at
    RhT = A([8, 128], BF16, at=Ll[1].at, toks=Ll[1].toks)
    PhiT = A([8, 64], F32)
    Psig = A([8, 64], F32)
    gC = A([8], F32)
    Fs = A([8], F32)
    mix = A([1024], BF16, at=Nn[1].at, toks=Nn[1].toks)
    mixT = A([8, 128], BF16, at=Yb.at, toks=Yb.toks)
    xn2T = mixT
    rl = A([64], F32)
    rtmp = A([80], F32)
    print("mixer sbuf el", ar.off, "of", ar.n, flush=True)

    tokK, tokB = Tok(), Tok()
    v3 = lambda ap: ap.rearrange("p (h d) -> p h d", h=8)

    print("MARK setup end", P.nadd, flush=True)
    def xload(tg):
        dma("sp", xt[tg % 2].ap, x[tg * 128:(tg + 1) * 128, :], [], [xt[tg % 2]])

    def head_steps(tg):
        i = tg % NT
        xT_c = xnT[tg % 2]
        xT_p = xnT[(tg + 1) % 2]
        xtile = xt[tg % 2]

        def s0():
            act(xnb.ap, xtile.ap, AF.Square, [xtile], [xnb, st8], accum=st8[:, 0:1])

        def s1():
            ts("dve", st8[:, 1:2], st8[:, 0:1], 1.0 / 1024, 1e-6, ALU.mult, ALU.add, [st8], [st8])
            act(st8[:, 2:3], st8[:, 1:2], AF.Ln, [st8], [st8])
            act(st8[:, 3:4], st8[:, 2:3], AF.Exp, [st8], [st8], scale=-0.5)

        def s2():
            ts("dve", xnb.ap, xtile.ap, st8[:, 3:4], None, ALU.mult, None, [xtile, st8], [xnb])

        def s3():
            for c in range(8):
                tr(pT[:, c * 128:(c + 1) * 128], xnb[:, c * 128:(c + 1) * 128], identb, [xnb, cb], [pT])

        def s4():
            cp("act", xT_c[:, :, 1:129], pT.ap.rearrange("p (c t) -> p c t", c=8), [pT], [xT_c])
            if i == 0:
                P.add("pool", lambda e, t=xT_c: e.memset(t[:, :, 0:1], 0.0), [], [xT_c])
            else:
                cp("dve", xT_c[:, :, 0:1], xT_p[:, :, 128:129], [xT_p], [xT_c])
        return [s0, s1, s2, s3, s4]

    def head(tg):
        for f in head_steps(tg):
            f()

    def rw_inproj(tg):
        xT = xnT[tg % 2]
        rkv = [S_[0], S_[1], S_[2]]
        for q3 in range(3):
            pcur = pb[1][:, 0:512]
            pprv = pb[1][:, 512:1024]
            for c in range(8):
                mm(pcur, xT[:, c, 1:129], Wrw[:, c, q3 * 512:(q3 + 1) * 512], c == 0, c == 7, [Wrw, xT], [pb[1]])
            for c in range(8):
                mm(pprv, xT[:, c, 0:128], Wrw[:, c, q3 * 512:(q3 + 1) * 512], c == 0, c == 7, [Wrw, xT], [pb[1]])
            cp("act", S_[3].ap, pcur, [pb[1]], [S_[3]])
            tt("dve", S_[4].ap, pprv, S_[3].ap, ALU.subtract, [pb[1], S_[3]], [S_[4]])
            tt("dve", S_[4].ap, S_[4].ap, bct[:, BC_MU + q3 * 512:BC_MU + (q3 + 1) * 512], ALU.mult, [S_[4], bct], [S_[4]])
            tt("dve", rkv[q3].ap, S_[4].ap, S_[3].ap, ALU.add, [S_[4], S_[3]], [rkv[q3]])
        cp("dve", Vb.ap, S_[2].ap, [S_[2]], [Vb])

    xload(0)
    head(0)
    rw_inproj(0)
    for s in range(NSEQ):
        P.add("dve", lambda e: e.memset(Hst.ap, 0.0), [], [Hst])
        P.add("dve", lambda e: e.memset(Hb.ap, 0.0), [], [Hb])
        for i in range(NT):
            tg = s * NT + i
            xT_c = xnT[tg % 2]
            xtile = xt[tg % 2]
            row0 = tg * 128
            if tg + 1 < NSEQ * NT:
                xload(tg + 1)
            cur = lambda c: xT_c[:, c, 1:129]
            prv = lambda c: xT_c[:, c, 0:128]

            print("MARK tile", tg, "inproj", P.nadd, flush=True)
            r_, k_, v_ = S_[0], S_[1], S_[2]
            print("MARK lora", P.nadd, flush=True)
            for q2 in range(2):
                c0 = 1536 + q2 * 128
                for c in range(8):
                    mm(pc[:, q2 * 256:q2 * 256 + 128], Wrw[:, c, c0:c0 + 128], cur(c), c == 0, c == 7, [Wrw, xT_c], [pc])
                for c in range(8):
                    mm(pc[:, q2 * 256 + 128:q2 * 256 + 256], Wrw[:, c, c0:c0 + 128], prv(c), c == 0, c == 7, [Wrw, xT_c], [pc])
            for j in range(4):
                for c in range(8):
                    mm(pb[0][:, j * 128:(j + 1) * 128], Wsb[:, c, j * 128:(j + 1) * 128], cur(c), c == 0, c == 7, [Wsb, xT_c], [pb[0]])
            for j in range(4):
                for c in range(8):
                    mm(pb[0][:, 512 + j * 128:512 + (j + 1) * 128], Wsb[:, c, 512 + j * 128:512 + (j + 1) * 128], cur(c), c == 0, c == 7, [Wsb, xT_c], [pb[0]])
            lo = S_[3]
            stK = Tile(st8[:, 8:32], [tokK])
            stB = Tile(st8[:, 32:40], [tokB])
            for q2 in range(2):
                ts("dve", lo[:, q2 * 128:(q2 + 1) * 128], pc[:, q2 * 256:q2 * 256 + 128], omm[:, q2:q2 + 1], None, ALU.mult, None, [pc, omm], [lo])
                stt("dve", lo[:, q2 * 128:(q2 + 1) * 128], pc[:, q2 * 256 + 128:q2 * 256 + 256], pkt[:, PK_MUL + q2:PK_MUL + q2 + 1],
                    lo[:, q2 * 128:(q2 + 1) * 128], ALU.mult, ALU.add, [pc, pkt, lo], [lo])
            act(lo[0:64, 256:384], lo[0:64, 0:128], AF.Exp, [lo], [lo], scale=2.0)
            act(lo[:, 384:512], lo[:, 128:256], AF.Exp, [lo], [lo], scale=-1.0)
            cp("act", qT.ap, pb[0][:, 0:512].rearrange("p (j t) -> p j t", j=4), [pb[0]], [qT])
            cp("act", kT[:, :, i * 128:(i + 1) * 128], pb[0][:, 512:1024].rearrange("p (j t) -> p j t", j=4), [pb[0]], [kT])
            kkn = S_[8]
            tt("dve", kkn.ap, k_.ap, bct[:, BC_KK:BC_KK + 512], ALU.mult, [k_, bct], [kkn])
            tt("dve", S_[9].ap, kkn.ap, kkn.ap, ALU.mult, [kkn], [S_[9]])
            red("dve", stK[:, 0:8], v3(S_[9].ap), ALU.add, [S_[9]], [stK])
            ts("dve", stK[:, 0:8], stK[:, 0:8], 1e-24, None, ALU.max, None, [stK], [stK])
            ts("dve", lo[0:64, 256:384], lo[0:64, 256:384], 1.0, None, ALU.add, None, [lo], [lo])
            recip(lo[0:64, 256:384], lo[0:64, 256:384], [lo], [lo])
            ts("dve", loT[0:64, 0, :], lo[0:64, 256:384], -2.0, 1.0, ALU.mult, ALU.add, [lo], [loT])
            cp("dve", loT[64:128, 0, :], lo[64:128, 0:128], [lo], [loT])
            ts("dve", lo[:, 384:512], lo[:, 384:512], 1.0, None, ALU.add, None, [lo], [lo])
            recip(lo[:, 384:512], lo[:, 384:512], [lo], [lo])
            cp("dve", loT[:, 1, :], lo[:, 384:512], [lo], [loT])
            mm(pb[2][:, 0:512], loT[0:64, 0, :], lwb[0:64, 0:512], True, True, [loT, lwb], [pb[2]])
            mm(pb[2][:, 512:1024], loT[64:128, 0, :], lwb[64:128, 0:512], True, True, [loT, lwb], [pb[2]])
            mm(pc[:, 0:512], loT[:, 1, :], lwb[:, 512:1024], True, True, [loT, lwb], [pc])
            act(stK[:, 8:16], stK[:, 0:8], AF.Ln, [stK], [stK])
            act(stK[:, 16:24], stK[:, 8:16], AF.Exp, [stK], [stK], scale=-0.5)
            g_ = S_[7]
            cp("act", g_.ap, pc[:, 0:512], [pc], [g_])
            for c in range(8):
                mm(pc[:, 0:512], cur(c), Wsb[:, c, 1024:1536], c == 0, c == 7, [Wsb, xT_c], [pc])
            lg = S_[4]
            a_ = S_[6]
            tt("dve", lg.ap, pb[2][:, 0:512], bct[:, BC_W0:BC_W0 + 512], ALU.add, [pb[2], bct], [lg])
            tt("dve", a_.ap, pb[2][:, 512:1024], bct[:, BC_A0:BC_A0 + 512], ALU.add, [pb[2], bct], [a_])
            act(lg.ap, lg.ap, AF.Exp, [lg], [lg], scale=-1.0)
            act(lg.ap, lg.ap, AF.Ln, [lg], [lg], bias=1.0)
            act(lg.ap, lg.ap, AF.Exp, [lg], [lg], scale=-1.0)
            act(a_.ap, a_.ap, AF.Exp, [a_], [a_], scale=-1.0)
            act(a_.ap, a_.ap, AF.Ln, [a_], [a_], bias=1.0)
            act(a_.ap, a_.ap, AF.Exp, [a_], [a_], scale=-1.0)
            cp("act", vs[:, i, :], pc[:, 0:512], [pc], [vs])
            tt("dve", v3(kkn.ap), v3(kkn.ap), stK[:, 16:24].unsqueeze(2).to_broadcast([128, 8, 64]), ALU.mult, [kkn, stK], [kkn])
            ts("dve", lg.ap, lg.ap, -0.6065306597126334, None, ALU.mult, None, [lg], [lg])
            mm(pb[2][:, 0:512], mIN, lg.ap, True, True, [pkt, lg], [pb[2]])
            cl = S_[5]
            cp("act", cl.ap, pb[2][:, 0:512], [pb[2]], [cl])
            km = S_[9]
            ts("dve", km.ap, a_.ap, -1.0, None, ALU.add, None, [a_], [km])
            tt("dve", km.ap, km.ap, bct[:, BC_KA:BC_KA + 512], ALU.mult, [km, bct], [km])
            ts("dve", km.ap, km.ap, 1.0, None, ALU.add, None, [km], [km])
            tt("dve", km.ap, km.ap, k_.ap, ALU.mult, [km, k_], [km])
            dump(lg, 2048, row0)
            dump(a_, 2560, row0)
            dump(g_, 3072, row0)
            dump(cl, 3584, row0)
            dump(kkn, 4096, row0)
            dump(km, 4608, row0)
            dump(r_, 5632, row0)
            dump(k_, 6144, row0)
            dump(v_, 6656, row0)
            for h in range(8):
                mm(pc[0:64, h:h + 1], cl[:, h * 64:(h + 1) * 64], pkt[:, PK_ELAST:PK_ELAST + 1], True, True, [cl, pkt], [pc])
            eA, eN, eP = S_[10], S_[3], S_[4]
            tt("dve", eA.ap, cl.ap, lg.ap, ALU.subtract, [cl, lg], [eA])
            act(eA.ap, eA.ap, AF.Exp, [eA], [eA])
            act(eN.ap, cl.ap, AF.Exp, [cl], [eN], scale=-1.0)
            act(eP.ap, cl.ap, AF.Exp, [cl], [eP])
            act(gC[0:64, :], pc[0:64, 0:8], AF.Exp, [pc], [gC])
            stt("dve", Atc.ap, kkn.ap, -1.0, eA.ap, ALU.mult, ALU.mult, [kkn, eA], [Atc])
            cp("dve", At[:, :, 0:64], v3(Atc.ap), [Atc], [At])
            tt("dve", a_.ap, kkn.ap, a_.ap, ALU.mult, [kkn, a_], [a_])
            tt("dve", Bt.ap, a_.ap, eN.ap, ALU.mult, [a_, eN], [Bt])
            tt("dve", Kt.ap, km.ap, eN.ap, ALU.mult, [km, eN], [Kt])
            tt("dve", Rt.ap, r_.ap, eP.ap, ALU.mult, [r_, eP], [Rt])
            bon = S_[10]
            tt("dve", S_[3].ap, r_.ap, km.ap, ALU.mult, [r_, km], [S_[3]])
            tt("dve", S_[3].ap, S_[3].ap, bct[:, BC_RK:BC_RK + 512], ALU.mult, [S_[3], bct], [S_[3]])
            red("dve", stB[:, 0:8], v3(S_[3].ap), ALU.add, [S_[3]], [stB])
            tt("dve", v3(bon.ap), v3(v_.ap), stB[:, 0:8].unsqueeze(2).to_broadcast([128, 8, 64]), ALU.mult, [v_, stB], [bon])

            print("MARK fmajor", P.nadd, flush=True)
            for hp in range(4):
                tr(pT[:, hp * 128:(hp + 1) * 128], Atc[:, hp * 128:(hp + 1) * 128], identb, [Atc, cb], [pT])
                tr(pT[:, 512 + hp * 128:512 + (hp + 1) * 128], Rt[:, hp * 128:(hp + 1) * 128], identb, [Rt, cb], [pT])
            cp("act", ART[:, :, 0, :], pT[:, 0:512].rearrange("p (a t) -> p a t", a=4), [pT], [ART])
            cp("act", ART[:, :, 1, :], pT[:, 512:1024].rearrange("p (a t) -> p a t", a=4), [pT], [ART])
            for hp in range(4):
                tr(pT[:, hp * 128:(hp + 1) * 128], Bt[:, hp * 128:(hp + 1) * 128], identb, [Bt, cb], [pT])
                tr(pT[:, 512 + hp * 128:512 + (hp + 1) * 128], Kt[:, hp * 128:(hp + 1) * 128], identb, [Kt, cb], [pT])
            cp("act", BT.ap, pT[:, 0:512].rearrange("p (a t) -> p a t", a=4), [pT], [BT])
            cp("act", KTt.ap, pT[:, 512:1024].rearrange("p (a t) -> p a t", a=4), [pT], [KTt])

            N0, L0 = Nn[0], Ll[0]
            for hg in range(2):
                for hh in range(4):
                    h = hg * 4 + hh
                    hp, b0 = h // 2, (h % 2) * 64
                    par, q = hh % 2, hh // 2
                    rhs2 = ART[b0:b0 + 64, hp, :, :]
                    o1 = par * 512 + q * 256
                    o3 = par * 512 + q * 128
                    mm(pb[0][:, o1:o1 + 256], BT[b0:b0 + 64, hp, :], rhs2, True, True, [BT, ART], [pb[0]])
                    mm(pb[1][:, o1:o1 + 256], KTt[b0:b0 + 64, hp, :], rhs2, True, True, [KTt, ART], [pb[1]])
                    mm(pb[2][:, o3:o3 + 128], ART[b0:b0 + 64, hp, 0, :], BT[b0:b0 + 64, hp, :], True, True, [BT, ART], [pb[2]])
                g1 = pb[0].ap.rearrange("p (par q two t) -> p par q two t", par=2, q=2, two=2)
                g2 = pb[1].ap.rearrange("p (par q two t) -> p par q two t", par=2, q=2, two=2)
                g3 = pb[2].ap.rearrange("p (par x) -> p par x", par=2)
                bSU = mSU.unsqueeze(1).to_broadcast([128, 2, 128])
                bIN = mIN.unsqueeze(1).to_broadcast([128, 2, 128])
                bLO = mLO.unsqueeze(1).to_broadcast([128, 2, 128])
                hv = lambda T_: T_.ap.rearrange("p (g q par) t -> p g par q t", g=2, q=2, par=2)
                for par in range(2):
                    tt("dve", hv(N0)[:, hg, par], g1[:, par, :, 0, :], bSU, ALU.mult, [pb[0], pkt], [N0])
                    tt("dve", hv(MrbT)[:, hg, par], g1[:, par, :, 1, :], bIN, ALU.mult, [pb[0], pkt], [MrbT])
                    tt("dve", hv(LakT)[:, hg, par], g2[:, par, :, 0, :], bSU, ALU.mult, [pb[1], pkt], [LakT])
                    tt("dve", hv(MrkT)[:, hg, par], g2[:, par, :, 1, :], bIN, ALU.mult, [pb[1], pkt], [MrkT])
                    tt("dve", hv(L0)[:, hg, par], g3[:, par, 0:256].rearrange("p (q t) -> p q t", q=2), bLO, ALU.mult, [pb[2], pkt], [L0])
            for h in range(8):
                mm(pc[:, h * 64:(h + 1) * 64], LakT[:, h, :], Vb[:, h * 64:(h + 1) * 64], True, True, [LakT, Vb], [pc])
            cp("act", At[:, :, 64:128], pc.ap.rearrange("p (h d) -> p h d", h=8), [pc], [At])
            print("MARK neumann", P.nadd, flush=True)
            Ycur = At
            Ncur, Lcur = N0, L0
            for bk in range(2):
                mm(pb[2][:, bk * 512:(bk + 1) * 512], identb, At[:, 4 * bk:4 * bk + 4, :], True, False, [cb, At], [pb[2]])
            hst_ = head_steps(tg + 1) if tg + 1 < NSEQ * NT else []
            for lvl in range(7):
                for h in range(8):
                    mm(pb[2][:, h * 128:(h + 1) * 128], Ncur[:, h, :], Ycur[:, h, :], False, lvl == 6, [Ncur, Ycur], [pb[2]])
                if lvl < 6:
                    Nnx, Lnx = Nn[(lvl + 1) % 2], Ll[(lvl + 1) % 2]
                    for h in range(8):
                        mm(pb[0][:, h * 128:(h + 1) * 128], Lcur[:, h, :], Ncur[:, h, :], True, True, [Lcur, Ncur], [pb[0]])
                    if lvl < 5:
                        for h in range(8):
                            mm(pb[1][:, h * 128:(h + 1) * 128], Ncur[:, h, :], Lcur[:, h, :], True, True, [Lcur, Ncur], [pb[1]])
                    cp("act", Yb.ap, pb[2].ap.rearrange("p (h t) -> p h t", h=8), [pb[2]], [Yb])
                    cp("dve", Nnx.ap, pb[0].ap.rearrange("p (h t) -> p h t", h=8), [pb[0]], [Nnx])
                    if lvl < 5:
                        cp("dve", Lnx.ap, pb[1].ap.rearrange("p (h t) -> p h t", h=8), [pb[1]], [Lnx])
                    Ycur, Ncur, Lcur = Yb, Nnx, Lnx
                if lvl < len(hst_):
                    hst_[lvl]()
            cp("act", Yb.ap, pb[2].ap.rearrange("p (h t) -> p h t", h=8), [pb[2]], [Yb])
            AU = Yb
            print("MARK rhat", P.nadd, flush=True)
            for h in range(8):
                mm(pb[0][0:64, h * 128:(h + 1) * 128], AU[:, h, 0:64], MrbT[:, h, :], True, False, [AU, MrbT], [pb[0]])
                mm(pb[0][0:64, h * 128:(h + 1) * 128], Rt[:, h * 64:(h + 1) * 64], identb, False, True, [Rt, cb], [pb[0]])
            cp("act", RhT[0:64, :, :], pb[0][0:64, :].rearrange("p (h t) -> p h t", h=8), [pb[0]], [RhT])
            for h in range(8):
                mm(pb[1][0:64, h * 64:(h + 1) * 64], AU[:, h, 0:64], Bt[:, h * 64:(h + 1) * 64], True, True, [AU, Bt], [pb[1]])
                mm(pb[1][0:64, 512 + h * 64:512 + (h + 1) * 64], Bt[:, h * 64:(h + 1) * 64], AU[:, h, 64:128], True, False, [AU, Bt], [pb[1]])
                mm(pb[1][0:64, 512 + h * 64:512 + (h + 1) * 64], Kt[:, h * 64:(h + 1) * 64], Vb[:, h * 64:(h + 1) * 64], False, True, [Kt, Vb], [pb[1]])
            tt("dve", PhiT[0:64, :, :], pb[1][0:64, 0:512].rearrange("p (h d) -> p h d", h=8),
               pkt[0:64, PK_ID:PK_ID + 64].unsqueeze(1).to_broadcast([64, 8, 64]), ALU.add, [pb[1], pkt], [PhiT])
            tt("dve", Psig[0:64, :, :], pb[1][0:64, 512:1024].rearrange("p (h d) -> p h d", h=8),
               gC[0:64, :].unsqueeze(2).to_broadcast([64, 8, 64]), ALU.mult, [pb[1], gC], [Psig])
            for h in range(8):
                mm(pc[:, h * 64:(h + 1) * 64], RhT[0:64, h, :], Hb[0:64, h, :], True, False, [RhT, Hb], [pc])
                mm(pc[:, h * 64:(h + 1) * 64], MrbT[:, h, :], AU[:, h, 64:128], False, False, [MrbT, AU], [pc])
                mm(pc[:, h * 64:(h + 1) * 64], MrkT[:, h, :], Vb[:, h * 64:(h + 1) * 64], False, True, [MrkT, Vb], [pc])
            orw = S_[3]
            cp("act", orw.ap, pc[:, 0:512], [pc], [orw])
            dump(orw, 5120, row0)
            for h in range(8):
                mm(pb[0][0:64, h * 64:(h + 1) * 64], PhiT[0:64, h, :], Hst[0:64, h, :], True, True, [PhiT, Hst], [pb[0]])
            tt("dve", Hst[0:64, :, :], pb[0][0:64, 0:512].rearrange("p (h d) -> p h d", h=8),
               gC[0:64, :].unsqueeze(2).to_broadcast([64, 8, 64]), ALU.mult, [pb[0], gC], [Hst])
            tt("dve", Hst[0:64, :, :], Hst[0:64, :, :], Psig[0:64, :, :], ALU.add, [Hst, Psig], [Hst])
            cp("dve", Hb[0:64, :, :], Hst[0:64, :, :], [Hst], [Hb])
            red("dve", st8[:, 40:48], v3(orw.ap), ALU.add, [orw], [st8])
            ts("dve", st8[:, 40:48], st8[:, 40:48], 1.0 / 64, None, ALU.mult, None, [st8], [st8])
            tt("dve", v3(orw.ap), v3(orw.ap), st8[:, 40:48].unsqueeze(2).to_broadcast([128, 8, 64]), ALU.subtract, [orw, st8], [orw])
            tt("dve", S_[4].ap, orw.ap, orw.ap, ALU.mult, [orw], [S_[4]])
            red("dve", st8[:, 48:56], v3(S_[4].ap), ALU.add, [S_[4]], [st8])
            ts("dve", st8[:, 48:56], st8[:, 48:56], 1.0 / 64, 64e-5, ALU.mult, ALU.add, [st8], [st8])
            act(st8[:, 56:64], st8[:, 48:56], AF.Ln, [st8], [st8])
            act(st8[:, 48:56], st8[:, 56:64], AF.Exp, [st8], [st8], scale=-0.5)
            tt("dve", v3(orw.ap), v3(orw.ap), st8[:, 48:56].unsqueeze(2).to_broadcast([128, 8, 64]), ALU.mult, [orw, st8], [orw])
            tt("dve", orw.ap, orw.ap, bct[:, BC_LNW:BC_LNW + 512], ALU.mult, [orw, bct], [orw])
            tt("dve", orw.ap, orw.ap, bct[:, BC_LNB:BC_LNB + 512], ALU.add, [orw, bct], [orw])
            tt("dve", orw.ap, orw.ap, bon.ap, ALU.add, [orw, bon], [orw])
            tt("dve", mix[:, 512:1024], orw.ap, g_.ap, ALU.mult, [orw, g_], [mix])

            print("MARK sb", P.nadd, flush=True)
            KSKIP = _os.environ.get("KSKIP", "")
            blk = lambda h: (h % 2) * 4 + h // 2

            h8 = lambda ap: ap.rearrange("p (h t) -> p h t", h=8)

            def sb_z(kb):
                pz = pb[kb % 2]
                for h in range(8):
                    hp, b0 = h // 2, (h % 2) * 64
                    mm(pz[:, blk(h) * 128:(blk(h) + 1) * 128], kT[b0:b0 + 64, hp, kb * 128:(kb + 1) * 128], qT[b0:b0 + 64, hp, :], True, True, [kT, qT], [pz])

            def sb_el(kb):
                par_ = kb % 2
                E_, Lp, pz = E2[par_], Lp2[par_], pb[par_]
                act(E_.ap, pz.ap, AF.Exp, [pz], [E_])
                if kb == i:
                    tt("dve", h8(E_.ap), h8(E_.ap), mSU.unsqueeze(1).to_broadcast([128, 8, 128]), ALU.mult, [E_, pkt], [E_])
                act(Lp.ap, E_.ap, AF.Ln, [E_], [Lp], bias=1.0)

            def sb_cum(kb):
                Lp = Lp2[kb % 2]
                mm(pb[2][:, 0:512], negtri, Lp[:, 0:512], True, False, [cb, Lp], [pb[2]])
                mm(pb[2][:, 512:1024], negtri, Lp[:, 512:1024], True, False, [cb, Lp], [pb[2]])
                for h in range(8):
                    hp, b0 = h // 2, (h % 2) * 64
                    mm(pb[2][:, blk(h) * 128:(blk(h) + 1) * 128], kT[b0:b0 + 64, hp, kb * 128:(kb + 1) * 128], qT[b0:b0 + 64, hp, :], False, True, [kT, qT], [pb[2]])
                if kb != kbs[0]:
                    for h in range(8):
                        mm(pTf[:, h:h + 1], Lp[:, blk(h) * 128:(blk(h) + 1) * 128], negtri[:, 0:1], True, True, [Lp, cb], [pTf])

            def sb_w(kb):
                ATt = AT2[kb % 2]
                act(ATt.ap, pb[2].ap, AF.Exp, [pb[2]], [ATt])
                if kb == i:
                    tt("dve", h8(ATt.ap), h8(ATt.ap), mSU.unsqueeze(1).to_broadcast([128, 8, 128]), ALU.mult, [ATt, pkt], [ATt])
                if kb != kbs[0]:
                    act(Fs.ap, pTf[:, 0:8], AF.Exp, [pTf], [Fs])

            def sb_pv(kb):
                ATt = AT2[kb % 2]
                for h in range(8):
                    mm(pc[:, h * 64:(h + 1) * 64], ATt[:, blk(h) * 128:(blk(h) + 1) * 128], vs[:, kb, h * 64:(h + 1) * 64], True, True, [ATt, vs], [pc])
                if kb == kbs[0]:
                    cp("dve", acc.ap, pc[:, 0:512], [pc], [acc])
                else:
                    tt("dve", v3(acc.ap), v3(acc.ap), Fs.ap.unsqueeze(2).to_broadcast([128, 8, 64]), ALU.mult, [acc, Fs], [acc])
                    tt("dve", acc.ap, acc.ap, pc[:, 0:512], ALU.add, [acc, pc], [acc])

            kbs = [kb for kb in range(i + 1) if not ("s" in KSKIP and kb > 0)]
            sb_z(kbs[0])
            sb_el(kbs[0])
            if len(kbs) > 1:
                sb_z(kbs[1])
            for n_, kb in enumerate(kbs):
                sb_cum(kb)
                if n_ + 2 < len(kbs):
                    sb_z(kbs[n_ + 2])
                if n_ + 1 < len(kbs):
                    sb_el(kbs[n_ + 1])
                sb_w(kb)
                sb_pv(kb)
            tt("dve", S_[4].ap, acc.ap, acc.ap, ALU.mult, [acc], [S_[4]])
            red("dve", st8[:, 40:48], v3(S_[4].ap), ALU.add, [S_[4]], [st8])
            ts("dve", st8[:, 40:48], st8[:, 40:48], 1.0 / 64, 1e-6, ALU.mult, ALU.add, [st8], [st8])
            act(st8[:, 56:64], st8[:, 40:48], AF.Ln, [st8], [st8])
            act(st8[:, 40:48], st8[:, 56:64], AF.Exp, [st8], [st8], scale=-0.5)
            tt("dve", v3(mix[:, 0:512]), v3(acc.ap), st8[:, 40:48].unsqueeze(2).to_broadcast([128, 8, 64]), ALU.mult, [acc, st8], [mix])

            print("MARK outproj", P.nadd, flush=True)
            for c in range(8):
                tr(pT[:, c * 128:(c + 1) * 128], mix[:, c * 128:(c + 1) * 128], identb, [mix, cb], [pT])
            cp("act", mixT.ap, pT.ap.rearrange("p (c t) -> p c t", c=8), [pT], [mixT])
            for half in range(2):
                for c in range(8):
                    mm(pb[0][:, half * 512:(half + 1) * 512], mixT[:, c, :], Wout[:, c, half * 512:(half + 1) * 512], c == 0, c == 7, [mixT, Wout], [pb[0]])
            ht = xtile
            tt("dve", ht.ap, pb[0].ap, xtile.ap, ALU.add, [pb[0], xtile], [ht])
            dma("sp", hbuf[row0:row0 + 128, :], ht.ap, [ht], [])
            if dbg:
                dma("sp", dbg_t[row0:row0 + 128, 0:1024], ht.ap, [ht], [], final=True)
            act(xn2.ap, ht.ap, AF.Square, [ht], [xn2, st8], accum=st8[:, 4:5])
            ts("dve", st8[:, 5:6], st8[:, 4:5], 1.0 / 1024, 1e-6, ALU.mult, ALU.add, [st8], [st8])
            act(st8[:, 6:7], st8[:, 5:6], AF.Ln, [st8], [st8])
            act(st8[:, 7:8], st8[:, 6:7], AF.Exp, [st8], [st8], scale=-0.5)
            stt("dve", xn2.ap, ht.ap, st8[:, 7:8], bct[:, BC_GFFN:BC_GFFN + 1024], ALU.mult, ALU.mult, [ht, st8, bct], [xn2])
            if tg + 1 < NSEQ * NT:
                rw_inproj(tg + 1)
            for c in range(8):
                tr(pT[:, c * 128:(c + 1) * 128], xn2[:, c * 128:(c + 1) * 128], identb, [xn2, cb], [pT])
            cp("act", xn2T.ap, pT.ap.rearrange("p (c t) -> p c t", c=8), [pT], [xn2T])
            dma("sp", x2d[:, :, row0:row0 + 128], xn2T.ap, [xn2T], [])
            for c in range(8):
                mm(pc[:, 0:36], xn2T[:, c, :], Wr[:, c, :], c == 0, c == 7, [xn2T, Wr], [pc])
            tt("dve", rl[:, 0:36], pc[:, 0:36], bct[:, BC_RB:BC_RB + 36], ALU.add, [pc, bct], [rl])
            red("dve", st8[:, 40:41], rl[:, 0:4], ALU.max, [rl], [st8])
            ts("dve", rtmp[:, 0:4], rl[:, 0:4], st8[:, 40:41], None, ALU.is_equal, None, [rl, st8], [rtmp])
            ts("dve", rtmp[:, 4:8], rl[:, 0:4], st8[:, 40:41], None, ALU.subtract, None, [rl, st8], [rtmp])
            act(rtmp[:, 4:8], rtmp[:, 4:8], AF.Exp, [rtmp], [rtmp, st8], accum=st8[:, 41:42])
            P.add("dve", lambda e: e.reciprocal(out=st8[:, 42:43], in_=st8[:, 41:42]), [st8], [st8])
            tt("dve", rtmp[:, 8:40].rearrange("p (g j) -> p g j", g=4), rl[:, 4:36].rearrange("p (g j) -> p g j", g=4),
               rtmp[:, 0:4].unsqueeze(2).to_broadcast([128, 4, 8]), ALU.mult, [rl, rtmp], [rtmp])
            red("dve", rtmp[:, 40:48], rtmp[:, 8:40].rearrange("p (g j) -> p j g", g=4), ALU.add, [rtmp], [rtmp])
            red("dve", st8[:, 43:44], rtmp[:, 40:48], ALU.max, [rtmp], [st8])
            ts("dve", rtmp[:, 48:56], rtmp[:, 40:48], st8[:, 43:44], None, ALU.is_equal, None, [rtmp, st8], [rtmp])
            stt("dve", rtmp[:, 56:64], rtmp[:, 48:56], -1e30, rtmp[:, 40:48], ALU.mult, ALU.add, [rtmp], [rtmp])
            red("dve", st8[:, 44:45], rtmp[:, 56:64], ALU.max, [rtmp], [st8])
            ts("dve", rtmp[:, 64:72], rtmp[:, 56:64], st8[:, 44:45], None, ALU.is_equal, None, [rtmp, st8], [rtmp])
            tt("dve", st8[:, 45:46], st8[:, 44:45], st8[:, 43:44], ALU.subtract, [st8], [st8])
            act(st8[:, 46:47], st8[:, 45:46], AF.Exp, [st8], [st8])
            ts("dve", st8[:, 47:48], st8[:, 46:47], 1.0, None, ALU.add, None, [st8], [st8])
            P.add("dve", lambda e: e.reciprocal(out=st8[:, 47:48], in_=st8[:, 47:48]), [st8], [st8])
            tt("dve", st8[:, 47:48], st8[:, 47:48], st8[:, 42:43], ALU.mult, [st8], [st8])
            tt("dve", st8[:, 46:47], st8[:, 46:47], st8[:, 47:48], ALU.mult, [st8], [st8])
            ts("dve", rtmp[:, 72:80], rtmp[:, 48:56], st8[:, 47:48], None, ALU.mult, None, [rtmp, st8], [rtmp])
            stt("dve", rtmp[:, 72:80], rtmp[:, 64:72], st8[:, 46:47], rtmp[:, 72:80], ALU.mult, ALU.add, [rtmp, st8], [rtmp])
            tt("dve", call[:, tg, :].rearrange("p (g j) -> p g j", g=4), rtmp[:, 0:4].unsqueeze(2).to_broadcast([128, 4, 8]),
               rtmp[:, 72:80].unsqueeze(1).to_broadcast([128, 4, 8]), ALU.mult, [rtmp], [call])
            if dbg:
                cp("dve", E_.ap, mix.ap, [mix], [E_])
                dma("sp", dbg_t[row0:row0 + 128, 1024:2048], E_.ap, [E_], [], final=True)

    print("MARK moe", P.nadd, flush=True)
    if _os.environ.get("KNOMOE"):
        P.stopped = True
    P.barrier()
    ar.off = mark_moe
    TB = min(2048, NTOK)
    NPASS = NTOK // TB
    TPB = TB // 128
    gfin = A([1024], F32)
    dma("sp", gfin.ap, bc[:, BC_GFIN:BC_GFIN + 1024], [], [gfin])
    x2 = A([8, TB], BF16)
    yh = []
    yacc = []
    for _t in range(TPB):
        h0_ = A([512], F32)
        at0 = ar.last_at
        h1_ = A([512], F32)
        yh.append((h0_, h1_))
        yacc.append(A([1024], F32, at=at0, toks=h0_.toks + h1_.toks))
    NWB = 2
    mstg = [A([2048], F32) for _ in range(4)]
    mi = 0
    Wg_ = [A([8, 256], BF16) for _ in range(NWB)]
    Wu_ = [A([8, 256], BF16) for _ in range(NWB)]
    Wd_ = [A([2, 1024], BF16) for _ in range(NWB)]
    hT = [A([2, TB], BF16) for _ in range(2)]
    sgt = [A([512], F32) for _ in range(2)]
    ysct = [A([512], F32) for _ in range(2)]
    st9 = A([8], F32)
    junk2 = A([1024], F32)
    ot = [A([1024], F32) for _ in range(2)]
    wi = 0
    for ps_ in range(NPASS):
        tok0 = ps_ * TB
        dma("sp", x2.ap, x2d[:, :, tok0:tok0 + TB], [], [x2])
        for t in range(TPB):
            dma("sp", yacc[t].ap, hbuf[tok0 + t * 128:tok0 + (t + 1) * 128, :], [], [yacc[t]])
        mi_ = [mi]
        k2_ = [0]
        yi_ = [0]
        tokY0, tokY1 = Tok(), Tok()

        def moe_load(ex):
            wb = ex % NWB
            for (dst, src, na_) in ((Wg_[wb], wg[ex], 8), (Wu_[wb], wu[ex], 8), (Wd_[wb], wd[ex], 2)):
                sgm = mstg[mi_[0] % len(mstg)]
                mi_[0] += 1
                sv = sgm.ap.rearrange("p (a b) -> p a b", a=na_)
                dma("sp", sv, src.rearrange("(c p) n -> p c n", p=128), [], [sgm])
                cp("pool", dst.ap, sv, [sgm], [dst])

        def moe_gu(ex):
            wb = ex % NWB
            hTe = hT[ex % 2]
            NB = min(512, TB)
            for hc in range(2):
                for tb in range(TB // NB):
                    pgu = pb[k2_[0] % 2]
                    sg_ = sgt[k2_[0] % 2]
                    k2_[0] += 1
                    for c in range(8):
                        mm(pgu[:, 0:NB], Wg_[wb][:, c, hc * 128:(hc + 1) * 128], x2[:, c, tb * NB:(tb + 1) * NB], c == 0, c == 7, [Wg_[wb], x2], [pgu])
                    for c in range(8):
                        mm(pgu[:, 512:512 + NB], Wu_[wb][:, c, hc * 128:(hc + 1) * 128], x2[:, c, tb * NB:(tb + 1) * NB], c == 0, c == 7, [Wu_[wb], x2], [pgu])
                    act(sg_[:, 0:NB], pgu[:, 0:NB], AF.Silu, [pgu], [sg_])
                    tt("dve", hTe[:, hc, tb * NB:(tb + 1) * NB], sg_[:, 0:NB], pgu[:, 512:512 + NB], ALU.mult, [sg_, pgu], [hTe])

        yring = [(pb[2][:, 0:512], Tile(pb[2][:, 0:512], [tokY0])), (pb[2][:, 512:1024], Tile(pb[2][:, 512:1024], [tokY1])),
                 (pc[:, 0:512], pc), (pTf[:, 0:512], pTf)]

        def moe_down(ex):
            wb = ex % NWB
            hTe = hT[ex % 2]
            for t in range(TPB):
                tgl = tok0 // 128 + t
                for half in range(2):
                    yp, ytok = yring[yi_[0] % 4]
                    yi_[0] += 1
                    for hc in range(2):
                        mm(yp, hTe[:, hc, t * 128:(t + 1) * 128], Wd_[wb][:, hc, half * 512:(half + 1) * 512],
                           hc == 0, hc == 1, [hTe, Wd_[wb]], [ytok])
                    stt("dve", yh[t][half].ap, yp, call[:, tgl, ex:ex + 1], yh[t][half].ap, ALU.mult, ALU.add,
                        [ytok, call, yh[t][half]], [yh[t][half]])

        moe_load(0)
        moe_load(1)
        moe_gu(0)
        for ex in range(32):
            if ex + 1 < 32:
                moe_gu(ex + 1)
            moe_down(ex)
            if ex + 2 < 32:
                moe_load(ex + 2)
        mi = mi_[0]
        for t in range(TPB):
            o_ = ot[t % 2]
            act(junk2.ap, yacc[t].ap, AF.Square, [yacc[t]], [junk2, st9], accum=st9[:, 0:1])
            ts("dve", st9[:, 1:2], st9[:, 0:1], 1.0 / 1024, 1e-6, ALU.mult, ALU.add, [st9], [st9])
            act(st9[:, 2:3], st9[:, 1:2], AF.Ln, [st9], [st9])
            act(st9[:, 3:4], st9[:, 2:3], AF.Exp, [st9], [st9], scale=-0.5)
            stt("dve", o_.ap, yacc[t].ap, st9[:, 3:4], gfin.ap, ALU.mult, ALU.mult, [yacc[t], st9, gfin], [o_])
            dma("sp", out[tok0 + t * 128:tok0 + (t + 1) * 128, :], o_.ap, [o_], [], final=True)
    stats = P.emit()
    print("ops", stats, "sbuf hi bytes", ar.hi * 2, flush=True)
    return nc


_CACHE = {}


def _pack(inp):
    f = np.float32
    g = lambda k: np.asarray(inp[k], dtype=f)
    pk = np.zeros((128, NPK), f)
    pk[:, PK_GMIX:PK_GMIX + 8] = g("norm_mix_g")[0].reshape(8, 128).T
    pk[:, PK_SBG:PK_SBG + 4] = g("sb_out_g")[0].reshape(4, 128).T
    mu = g("shift_mu")[0]
    pk[:, PK_MUL] = mu[1536:1664]
    pk[:, PK_MUL + 1] = mu[1664:1792]
    pk[127, PK_ELAST] = 1.0
    idx = np.arange(128)
    pk[:, PK_ID:PK_ID + 128] = np.eye(128, dtype=f)
    pk[:, PK_SU:PK_SU + 128] = (idx[:, None] < idx[None, :]).astype(f)
    pk[:, PK_IN:PK_IN + 128] = (idx[:, None] <= idx[None, :]).astype(f)
    pk[:, PK_LO:PK_LO + 128] = (idx[None, :] < idx[:, None]).astype(f)
    pk[:, PK_NT:PK_NT + 128] = -(idx[:, None] >= idx[None, :]).astype(f)
    row = np.zeros((NBC,), f)
    row[BC_MU:BC_MU + 1536] = mu[0:1536]
    row[BC_W0:BC_W0 + 512] = g("rw_w0")[0]
    row[BC_A0:BC_A0 + 512] = g("rw_a0")[0]
    row[BC_KK:BC_KK + 512] = g("rw_k_k")[0]
    row[BC_KA:BC_KA + 512] = g("rw_k_a")[0]
    row[BC_RK:BC_RK + 512] = g("rw_r_k")[0].reshape(512)
    row[BC_LNW:BC_LNW + 512] = g("rw_ln_w")[0]
    row[BC_LNB:BC_LNB + 512] = g("rw_ln_b")[0]
    row[BC_GFFN:BC_GFFN + 1024] = g("norm_ffn_g")[0]
    row[BC_RB:BC_RB + 4] = g("router_grp_b")[0]
    row[BC_RB + 4:BC_RB + 36] = g("router_exp_b")[0]
    row[BC_GFIN:BC_GFIN + 1024] = g("final_norm_g")
    bcm = np.ascontiguousarray(np.broadcast_to(row[None, :], (128, NBC)))
    lwm = np.concatenate([np.concatenate([g("rw_w2")[0], g("rw_a2")[0]], axis=0), g("rw_g2")[0]], axis=1)
    wrm = np.concatenate([g("router_grp_w")[0], g("router_exp_w")[0]], axis=1)
    return dict(w_in=g("w_in")[0], w_out=g("w_out")[0], wr=np.ascontiguousarray(wrm), lw=np.ascontiguousarray(lwm),
                wg=g("exp_w_gate")[0], wu=g("exp_w_up")[0], wd=g("exp_w_down")[0], pk=pk, bc=bcm)


def run(inputs, ncores, dbg=False):
    x = np.asarray(inputs["x"], dtype=np.float32)
    B, S, D = x.shape
    nseq = B // ncores
    key = (S, nseq, dbg)
    if key not in _CACHE:
        _CACHE[key] = build(S, nseq, dbg)
    nc = _CACHE[key]
    shared = _pack(inputs)
    in_maps = []
    for c in range(ncores):
        m = dict(shared)
        m["x"] = np.ascontiguousarray(x[c * nseq:(c + 1) * nseq].reshape(nseq * S, D))
        in_maps.append(m)
    res = run_bass_kernel_spmd(nc, in_maps, core_ids=list(range(ncores)))
    out = np.stack([r["out"].reshape(nseq, S, D) for r in res.results], axis=0).reshape(B, S, D)
    if dbg:
        return out, [r["dbg"] for r in res.results]
    return out


def kernel(**inputs):
    return run(inputs, 8).astype(np.float32)
```

```python
import numpy as np
import ml_dtypes
import concourse.bass as bass
import concourse.mybir as mybir
from concourse.bass_utils import run_bass_kernel_spmd

F32 = mybir.dt.float32
BF16 = mybir.dt.bfloat16
AF = mybir.ActivationFunctionType
ALU = mybir.AluOpType
AX = mybir.AxisListType

PK_GMIX, PK_SBG, PK_MUL, PK_ELAST, PK_ID, PK_SU, PK_IN, PK_LO, PK_NT, NPK = 0, 8, 12, 14, 16, 144, 272, 400, 528, 656
BC_MU, BC_W0, BC_A0, BC_KK, BC_KA, BC_RK, BC_LNW, BC_LNB, BC_GFFN, BC_RB, BC_GFIN, NBC = (
    0, 1536, 2048, 2560, 3072, 3584, 4096, 4608, 5120, 6144, 6192, 7216)
BC_MIX = 6192


import os as _osmod
STRICT = bool(_osmod.environ.get("KSTRICT"))


class Tok:
    __slots__ = ("last_w", "readers")

    def __init__(self):
        self.last_w = None
        self.readers = []


class Tile:
    def __init__(self, ap, toks=None):
        self.ap = ap
        self.toks = toks if toks is not None else [Tok()]

    def __getitem__(self, k):
        return self.ap[k]


class Prog:
    ENGS = ("pe", "act", "dve", "pool", "sp")
    NDMA = 40

    def __init__(self, nc):
        self.nc = nc
        self.ops = {e: [] for e in self.ENGS}
        self.waited = {e: {} for e in self.ENGS}
        self.ndma = 0
        self.dma_last = [None] * self.NDMA
        self.final_dma = []
        self.last_compute = {e: None for e in self.ENGS}

    def _need(self, eng, ref, waits):
        kind, key, val = ref
        w = self.waited[eng]
        k = (kind, key)
        if w.get(k, -1) >= val:
            return
        w[k] = val
        waits.append(ref)
        if kind == "eng":
            self.ops[key][val]["signal"] = True

    def add(self, eng, fn, reads=(), writes=(), dma=False, final=False):
        import os
        self.nadd = getattr(self, "nadd", 0) + 1
        if self.nadd > int(os.environ.get("KSTOP", "100000000")) or getattr(self, "stopped", False):
            return None
        reads = [k for t in reads for k in t.toks]
        writes = [k for t in writes for k in t.toks]
        deps = []
        for t in reads:
            if t.last_w is not None:
                deps.append((t.last_w, True))
        for t in writes:
            if t.last_w is not None:
                deps.append((t.last_w, False))
            for r in t.readers:
                deps.append((r, False))
        waits = []
        for ref, raw in deps:
            kind, key, val = ref
            if kind == "eng" and key == eng and not dma:
                if eng == "pe" or (not raw and not STRICT):
                    continue
            self._need(eng, ref, waits)
        idx = len(self.ops[eng])
        op = {"fn": fn, "waits": waits, "signal": False, "dma": None}
        if dma:
            slot = self.ndma % self.NDMA
            val = 16 * (self.ndma // self.NDMA + 1)
            self.ndma += 1
            if self.dma_last[slot] is not None:
                self._need(eng, self.dma_last[slot], waits)
            myref = ("dma", slot, val)
            self.dma_last[slot] = myref
            op["dma"] = (slot, val)
            if final:
                self.final_dma.append(myref)
        else:
            myref = ("eng", eng, idx)
            self.last_compute[eng] = myref
        self.ops[eng].append(op)
        for t in reads:
            t.readers.append(myref)
        for t in writes:
            t.last_w = myref
            t.readers = []
        return myref

    def barrier(self):
        refs = [r for r in self.last_compute.values() if r is not None]
        refs += [r for r in self.dma_last if r is not None]
        for e in self.ENGS:
            waits = []
            for ref in refs:
                if ref[0] == "eng" and ref[1] == e:
                    continue
                self._need(e, ref, waits)
            if waits:
                self.ops[e].append({"fn": None, "waits": waits, "signal": False, "dma": None})

    def emit(self):
        nc = self.nc
        fw = []
        for ref in self.final_dma:
            self._need("sp", ref, fw)
        if fw:
            self.ops["sp"].append({"fn": None, "waits": fw, "signal": False, "dma": None})
        ordinal = {}
        for e in self.ENGS:
            c = 0
            for i, op in enumerate(self.ops[e]):
                if op["signal"]:
                    c += 1
                    ordinal[(e, i)] = c
            print("SEMMAX", e, c, flush=True)
        from contextlib import ExitStack
        with ExitStack() as st:
            esem = {e: st.enter_context(nc.semaphore(f"s_{e}")) for e in self.ENGS}
            dsem = [st.enter_context(nc.semaphore(f"d_{i}")) for i in range(self.NDMA)]
            block = st.enter_context(nc.Block())

            def run(e, engobj):
                for i, op in enumerate(self.ops[e]):
                    for kind, key, val in op["waits"]:
                        if kind == "eng":
                            engobj.wait_ge(esem[key], ordinal[(key, val)])
                        else:
                            engobj.wait_ge(dsem[key], val)
                    if op["fn"] is None:
                        continue
                    ins = op["fn"](engobj)
                    if op["dma"] is not None:
                        ins.then_inc(dsem[op["dma"][0]], 16)
                    elif op["signal"]:
                        ins.then_inc(esem[e], 1)

            block.tensor(lambda eng: run("pe", eng))
            block.scalar(lambda eng: run("act", eng))
            block.vector(lambda eng: run("dve", eng))
            block.gpsimd(lambda eng: run("pool", eng))
            block.sync(lambda eng: run("sp", eng))
        return {e: len(self.ops[e]) for e in self.ENGS}


class Arena:
    def __init__(self, nc, nbytes):
        self.n = nbytes // 2
        self.t = nc.alloc_sbuf_tensor("arena", [128, self.n], BF16)
        self.off = 0
        self.hi = 0

    def alloc(self, shape, dt, at=None, toks=None):
        n = int(np.prod(shape))
        el = n * (2 if dt == F32 else 1)
        el = (el + 15) // 16 * 16
        if at is None:
            assert self.off + el <= self.n, f"SBUF arena overflow {self.off + el} > {self.n}"
            at = self.off
            self.off += el
            self.hi = max(self.hi, self.off)
        ap = self.t[:, at:at + el]
        self.last_at = at
        if dt == F32:
            ap = ap.bitcast(F32)
        ap = ap[:, 0:n]
        if len(shape) == 2:
            ap = ap.rearrange("p (a b) -> p a b", a=shape[0], b=shape[1])
        elif len(shape) == 3:
            ap = ap.rearrange("p (a b c) -> p a b c", a=shape[0], b=shape[1], c=shape[2])
        return Tile(ap, toks)


def build(S, NSEQ, dbg=False):
    import os as _os
    NT = S // 128
    NTOK = S * NSEQ
    NTT = NTOK // 128
    nc = bass.Bass("TRN2", target_bir_lowering=False)
    din = lambda name, shape, dt=F32: nc.dram_tensor(name, shape, dt, kind="ExternalInput").ap()
    x = din("x", [NTOK, 1024])
    w_in = din("w_in", [1024, 3328])
    w_out = din("w_out", [1024, 1024])
    wr = din("wr", [1024, 36])
    lw = din("lw", [128, 1024])
    wg = din("wg", [32, 1024, 256])
    wu = din("wu", [32, 1024, 256])
    wd = din("wd", [32, 256, 1024])
    pk = din("pk", [128, NPK])
    bc = din("bc", [128, NBC])
    out = nc.dram_tensor("out", [NTOK, 1024], F32, kind="ExternalOutput").ap()
    hbuf = nc.dram_tensor("hbuf", [NTOK, 1024], F32).ap()
    x2d = nc.dram_tensor("x2d", [128, 8, NTOK], BF16).ap()
    dbg_t = nc.dram_tensor("dbg", [NTOK, 8192], F32, kind="ExternalOutput").ap() if dbg else None

    P = Prog(nc)
    ar = Arena(nc, 212800)
    A = ar.alloc

    pb = [Tile(nc.alloc_psum_tensor(f"pb{i}", [128, 1024], F32)[:]) for i in range(3)]
    pc = Tile(nc.alloc_psum_tensor("pc", [128, 512], F32)[:])
    pT = Tile(nc.alloc_psum_tensor("pT", [128, 1024], BF16)[:])

    pTf = Tile(pT.ap.bitcast(F32), pT.toks)

    def mm(o, lhsT, rhs, start, stop, r, w):
        P.add("pe", lambda e: e.matmul(o, lhsT=lhsT, rhs=rhs, start=start, stop=stop, skip_group_check=True), r, w)

    def tr(o, in_, ident, r, w):
        P.add("pe", lambda e: e.transpose(out=o, in_=in_, identity=ident), r, w)

    def tt(eng, o, a, b, op, r, w):
        P.add(eng, lambda e: e.tensor_tensor(out=o, in0=a, in1=b, op=op), r, w)

    def ts(eng, o, a, s1, s2, op0, op1, r, w):
        if s2 is None:
            P.add(eng, lambda e: e.tensor_scalar(out=o, in0=a, scalar1=s1, scalar2=None, op0=op0), r, w)
        else:
            P.add(eng, lambda e: e.tensor_scalar(out=o, in0=a, scalar1=s1, scalar2=s2, op0=op0, op1=op1), r, w)

    def stt(eng, o, a, s, b, op0, op1, r, w):
        P.add(eng, lambda e: e.scalar_tensor_tensor(out=o, in0=a, scalar=s, in1=b, op0=op0, op1=op1), r, w)

    def act(o, in_, func, r, w, bias=0.0, scale=1.0, accum=None):
        if accum is None:
            P.add("act", lambda e: e.activation(out=o, in_=in_, func=func, bias=bias, scale=scale), r, w)
        else:
            P.add("act", lambda e: e.activation(out=o, in_=in_, func=func, bias=bias, scale=scale, accum_out=accum), r, w)

    def cp(eng, o, in_, r, w):
        if eng == "act":
            P.add("act", lambda e: e.activation(out=o, in_=in_, func=AF.Copy), r, w)
        else:
            P.add(eng, lambda e: e.tensor_copy(out=o, in_=in_), r, w)

    def recip(o, in_, r, w):
        P.add("dve", lambda e: e.reciprocal(out=o, in_=in_), r, w)

    def red(eng, o, in_, op, r, w):
        P.add(eng, lambda e: e.tensor_reduce(out=o, in_=in_, axis=AX.X, op=op), r, w)

    def dma(eng, o, in_, r, w, final=False):
        P.add(eng, lambda e: e.dma_start(out=o, in_=in_), r, w, dma=True, final=final)

    def dump(tile, col, row0, n=512):
        if dbg:
            dma("sp", dbg_t[row0:row0 + 128, col:col + n], tile.ap, [tile], [], final=True)

    def rsqrt_small(o, in_, r, w, tmp):
        act(tmp, in_, AF.Ln, r, [tmp_tok(tmp)])
        act(o, tmp, AF.Exp, [tmp_tok(tmp)], w, scale=-0.5)

    def tmp_tok(t):
        return t

    call = A([NTT, 32], F32)
    pkt = A([NPK], F32)
    mark_moe = ar.off
    bct = A([BC_MIX], F32)
    dma("sp", pkt.ap, pk, [], [pkt])
    dma("sp", bct.ap, bc[:, 0:BC_MIX], [], [bct])
    ident_f = pkt[:, PK_ID:PK_ID + 128]
    mSU = pkt[:, PK_SU:PK_SU + 128]
    mIN = pkt[:, PK_IN:PK_IN + 128]
    mLO = pkt[:, PK_LO:PK_LO + 128]
    cb = A([4, 128], BF16)
    cp("dve", cb[:, 0, :], ident_f, [pkt], [cb])
    cp("dve", cb[:, 1, :], pkt[:, PK_NT:PK_NT + 128], [pkt], [cb])
    identb = cb[:, 0, :]
    negtri = cb[:, 1, :]
    omm = A([2], F32)
    ts("dve", omm.ap, pkt[:, PK_MUL:PK_MUL + 2], -1.0, 1.0, ALU.mult, ALU.add, [pkt], [omm])

    Wsb = A([8, 1536], BF16)
    Wrw = A([8, 1792], BF16)
    Wout = A([8, 1024], BF16)
    Wr = A([8, 36], BF16)
    lwb = A([1024], BF16)
    kT = A([4, S], BF16)
    vs = A([NT, 512], BF16)
    Hst = A([8, 64], F32)
    Hb = A([8, 64], BF16)
    mark = ar.off

    stg = [A([8, 256], F32) for _ in range(2)]
    w_in_v = w_in.rearrange("(c p) n -> p c n", p=128)
    w_out_v = w_out.rearrange("(c p) n -> p c n", p=128)
    si = 0
    for c0 in range(0, 3328, 256):
        cw = 256
        sg = stg[si % 2]
        si += 1
        dma("sp", sg[:, :, 0:cw], w_in_v[:, :, c0:c0 + cw], [], [sg])
        for c in range(8):
            eng = "dve"
            if c0 < 1536:
                dst = Wsb[:, c, c0:c0 + cw]
                dt_ = Wsb
            else:
                dst = Wrw[:, c, c0 - 1536:c0 - 1536 + cw]
                dt_ = Wrw
            if c0 < 512:
                ts(eng, dst, sg[:, c, 0:cw], pkt[:, PK_GMIX + c:PK_GMIX + c + 1], 0.125, ALU.mult, ALU.mult, [sg, pkt], [dt_])
            else:
                ts(eng, dst, sg[:, c, 0:cw], pkt[:, PK_GMIX + c:PK_GMIX + c + 1], None, ALU.mult, None, [sg, pkt], [dt_])
    for c0 in range(0, 1024, 256):
        sg = stg[si % 2]
        si += 1
        dma("sp", sg.ap, w_out_v[:, :, c0:c0 + 256], [], [sg])
        for c in range(8):
            eng = "dve"
            if c < 4:
                ts(eng, Wout[:, c, c0:c0 + 256], sg[:, c, :], pkt[:, PK_SBG + c:PK_SBG + c + 1], None, ALU.mult, None, [sg, pkt], [Wout])
            else:
                cp(eng, Wout[:, c, c0:c0 + 256], sg[:, c, :], [sg], [Wout])
    sg = stg[si % 2]
    si += 1
    dma("sp", sg[:, :, 0:36], wr.rearrange("(c p) n -> p c n", p=128), [], [sg])
    cp("dve", Wr.ap, sg[:, :, 0:36], [sg], [Wr])
    sg = stg[si % 2]
    si += 1
    dma("sp", sg[:, 0:4, :], lw.rearrange("p (a b) -> p a b", a=4), [], [sg])
    cp("dve", lwb.ap.rearrange("p (a b) -> p a b", a=4), sg[:, 0:4, :], [sg], [lwb])
    P.barrier()
    ar.off = mark

    xt = [A([1024], F32) for _ in range(2)]
    st8 = A([64], F32)
    xnb = A([1024], BF16)
    xnT = [A([8, 129], BF16) for _ in range(2)]
    qT = A([4, 128], BF16)
    S_ = []
    for _ in range(11):
        S_.append(A([512], F32))
        S_[-1].at = ar.last_at
    al2 = lambda sl, shape, dt: A(shape, dt, at=S_[sl[0]].at, toks=[k for j in sl for k in S_[j].toks])
    E2 = [al2([5, 6], [1024], F32), al2([3, 4], [1024], F32)]
    X2 = [al2([7, 8], [1024], F32), al2([9, 10], [1024], F32)]
    Lp2 = [al2([0], [1024], BF16)]
    AT2 = [al2([1], [1024], BF16)]
    acc = al2([2], [512], F32)
    xn2 = al2([7], [1024], BF16)
    E_ = E2[0]
    Atc = A([512], BF16)
    At = A([8, 128], BF16)
    Bt = A([512], BF16)
    Kt = A([512], BF16)
    Rt = A([512], BF16)
    Vb = A([512], BF16)
    loT = A([2, 128], BF16)
    ART = A([4, 2, 128], BF16)
    BT = A([4, 128], BF16)
    KTt = A([4, 128], BF16)
    Nn = []
    for _ in range(2):
        Nn.append(A([8, 128], BF16))
        Nn[-1].at = ar.last_at
    Ll = []
    for _ in range(2):
        Ll.append(A([8, 128], BF16))
        Ll[-1].at = ar.last_at
    Lp2.append(A([1024], BF16, at=Nn[0].at, toks=Nn[0].toks))
    AT2.append(A([1024], BF16, at=Ll[0].at, toks=Ll[0].toks))
    MrbT = A([8, 128], BF16)
    LakT = A([8, 128], BF16)
    MrkT = A([8, 128], BF16)
    Yb = A([8, 128], BF16)
    Yb.at = ar.last_at
    RhT = A([8, 128], BF16, at=Ll[1].at, toks=Ll[1].toks)
    PhiT = A([8, 64], F32)
    Psig = A([8, 64], F32)
    gC = A([8], F32)
    Fs = A([8], F32)
    mix = A([1024], BF16, at=Nn[1].at, toks=Nn[1].toks)
    mixT = A([8, 128], BF16, at=Yb.at, toks=Yb.toks)
    xn2T = mixT
    rl = A([64], F32)
    rtmp = A([80], F32)
    print("mixer sbuf el", ar.off, "of", ar.n, flush=True)

    tokK, tokB = Tok(), Tok()
    v3 = lambda ap: ap.rearrange("p (h d) -> p h d", h=8)

    print("MARK setup end", P.nadd, flush=True)
    def xload(tg):
        dma("sp", xt[tg % 2].ap, x[tg * 128:(tg + 1) * 128, :], [], [xt[tg % 2]])

    def head_steps(tg):
        i = tg % NT
        xT_c = xnT[tg % 2]
        xT_p = xnT[(tg + 1) % 2]
        xtile = xt[tg % 2]

        def s0():
            act(xnb.ap, xtile.ap, AF.Square, [xtile], [xnb, st8], accum=st8[:, 0:1])

        def s1():
            ts("dve", st8[:, 1:2], st8[:, 0:1], 1.0 / 1024, 1e-6, ALU.mult, ALU.add, [st8], [st8])
            act(st8[:, 2:3], st8[:, 1:2], AF.Ln, [st8], [st8])
            act(st8[:, 3:4], st8[:, 2:3], AF.Exp, [st8], [st8], scale=-0.5)

        def s2():
            ts("dve", xnb.ap, xtile.ap, st8[:, 3:4], None, ALU.mult, None, [xtile, st8], [xnb])

        def s3():
            for c in range(8):
                tr(pT[:, c * 128:(c + 1) * 128], xnb[:, c * 128:(c + 1) * 128], identb, [xnb, cb], [pT])

        def s4():
            cp("act", xT_c[:, :, 1:129], pT.ap.rearrange("p (c t) -> p c t", c=8), [pT], [xT_c])
            if i == 0:
                P.add("pool", lambda e, t=xT_c: e.memset(t[:, :, 0:1], 0.0), [], [xT_c])
            else:
                cp("dve", xT_c[:, :, 0:1], xT_p[:, :, 128:129], [xT_p], [xT_c])
        return [s0, s1, s2, s3, s4]

    def head(tg):
        for f in head_steps(tg):
            f()

    def rw_inproj(tg):
        xT = xnT[tg % 2]
        rkv = [S_[0], S_[1], S_[2]]
        for q3 in range(3):
            pcur = pb[1][:, 0:512]
            pprv = pb[1][:, 512:1024]
            for c in range(8):
                mm(pcur, xT[:, c, 1:129], Wrw[:, c, q3 * 512:(q3 + 1) * 512], c == 0, c == 7, [Wrw, xT], [pb[1]])
            for c in range(8):
                mm(pprv, xT[:, c, 0:128], Wrw[:, c, q3 * 512:(q3 + 1) * 512], c == 0, c == 7, [Wrw, xT], [pb[1]])
            cp("act", S_[3].ap, pcur, [pb[1]], [S_[3]])
            tt("dve", S_[4].ap, pprv, S_[3].ap, ALU.subtract, [pb[1], S_[3]], [S_[4]])
            tt("dve", S_[4].ap, S_[4].ap, bct[:, BC_MU + q3 * 512:BC_MU + (q3 + 1) * 512], ALU.mult, [S_[4], bct], [S_[4]])
            tt("dve", rkv[q3].ap, S_[4].ap, S_[3].ap, ALU.add, [S_[4], S_[3]], [rkv[q3]])
        cp("dve", Vb.ap, S_[2].ap, [S_[2]], [Vb])

    xload(0)
    head(0)
    rw_inproj(0)
    for s in range(NSEQ):
        P.add("dve", lambda e: e.memset(Hst.ap, 0.0), [], [Hst])
        P.add("dve", lambda e: e.memset(Hb.ap, 0.0), [], [Hb])
        for i in range(NT):
            tg = s * NT + i
            xT_c = xnT[tg % 2]
            xtile = xt[tg % 2]
            row0 = tg * 128
            if tg + 1 < NSEQ * NT:
                xload(tg + 1)
            cur = lambda c: xT_c[:, c, 1:129]
            prv = lambda c: xT_c[:, c, 0:128]

            print("MARK tile", tg, "inproj", P.nadd, flush=True)
            r_, k_, v_ = S_[0], S_[1], S_[2]
            print("MARK lora", P.nadd, flush=True)
            for q2 in range(2):
                c0 = 1536 + q2 * 128
                for c in range(8):
                    mm(pc[:, q2 * 256:q2 * 256 + 128], Wrw[:, c, c0:c0 + 128], cur(c), c == 0, c == 7, [Wrw, xT_c], [pc])
                for c in range(8):
                    mm(pc[:, q2 * 256 + 128:q2 * 256 + 256], Wrw[:, c, c0:c0 + 128], prv(c), c == 0, c == 7, [Wrw, xT_c], [pc])
            for j in range(4):
                for c in range(8):
                    mm(pb[0][:, j * 128:(j + 1) * 128], Wsb[:, c, j * 128:(j + 1) * 128], cur(c), c == 0, c == 7, [Wsb, xT_c], [pb[0]])
            for j in range(4):
                for c in range(8):
                    mm(pb[0][:, 512 + j * 128:512 + (j + 1) * 128], Wsb[:, c, 512 + j * 128:512 + (j + 1) * 128], cur(c), c == 0, c == 7, [Wsb, xT_c], [pb[0]])
            lo = S_[3]
            stK = Tile(st8[:, 8:32], [tokK])
            stB = Tile(st8[:, 32:40], [tokB])
            for q2 in range(2):
                ts("dve", lo[:, q2 * 128:(q2 + 1) * 128], pc[:, q2 * 256:q2 * 256 + 128], omm[:, q2:q2 + 1], None, ALU.mult, None, [pc, omm], [lo])
                stt("dve", lo[:, q2 * 128:(q2 + 1) * 128], pc[:, q2 * 256 + 128:q2 * 256 + 256], pkt[:, PK_MUL + q2:PK_MUL + q2 + 1],
                    lo[:, q2 * 128:(q2 + 1) * 128], ALU.mult, ALU.add, [pc, pkt, lo], [lo])
            act(lo[0:64, 256:384], lo[0:64, 0:128], AF.Exp, [lo], [lo], scale=2.0)
            act(lo[0:64, 256:384], lo[0:64, 256:384], AF.Ln, [lo], [lo], bias=1.0)
            act(lo[0:64, 256:384], lo[0:64, 256:384], AF.Exp, [lo], [lo], scale=-1.0)
            act(lo[:, 384:512], lo[:, 128:256], AF.Exp, [lo], [lo], scale=-1.0)
            act(lo[:, 384:512], lo[:, 384:512], AF.Ln, [lo], [lo], bias=1.0)
            act(loT[:, 1, :], lo[:, 384:512], AF.Exp, [lo], [loT], scale=-1.0)
            cp("act", qT.ap, pb[0][:, 0:512].rearrange("p (j t) -> p j t", j=4), [pb[0]], [qT])
            cp("act", kT[:, :, i * 128:(i + 1) * 128], pb[0][:, 512:1024].rearrange("p (j t) -> p j t", j=4), [pb[0]], [kT])
            kkn = S_[8]
            tt("dve", kkn.ap, k_.ap, bct[:, BC_KK:BC_KK + 512], ALU.mult, [k_, bct], [kkn])
            tt("dve", S_[9].ap, kkn.ap, kkn.ap, ALU.mult, [kkn], [S_[9]])
            red("dve", stK[:, 0:8], v3(S_[9].ap), ALU.add, [S_[9]], [stK])
            ts("dve", stK[:, 0:8], stK[:, 0:8], 1e-24, None, ALU.max, None, [stK], [stK])
            ts("dve", loT[0:64, 0, :], lo[0:64, 256:384], -2.0, 1.0, ALU.mult, ALU.add, [lo], [loT])
            cp("dve", loT[64:128, 0, :], lo[64:128, 0:128], [lo], [loT])
            mm(pb[2][:, 0:512], loT[0:64, 0, :], lwb[0:64, 0:512], True, True, [loT, lwb], [pb[2]])
            mm(pb[2][:, 512:1024], loT[64:128, 0, :], lwb[64:128, 0:512], True, True, [loT, lwb], [pb[2]])
            mm(pc[:, 0:512], loT[:, 1, :], lwb[:, 512:1024], True, True, [loT, lwb], [pc])
            act(stK[:, 8:16], stK[:, 0:8], AF.Ln, [stK], [stK])
            act(stK[:, 16:24], stK[:, 8:16], AF.Exp, [stK], [stK], scale=-0.5)
            g_ = S_[7]
            cp("act", g_.ap, pc[:, 0:512], [pc], [g_])
            for c in range(8):
                mm(pc[:, 0:512], cur(c), Wsb[:, c, 1024:1536], c == 0, c == 7, [Wsb, xT_c], [pc])
            lg = S_[4]
            a_ = S_[6]
            tt("dve", lg.ap, pb[2][:, 0:512], bct[:, BC_W0:BC_W0 + 512], ALU.add, [pb[2], bct], [lg])
            tt("dve", a_.ap, pb[2][:, 512:1024], bct[:, BC_A0:BC_A0 + 512], ALU.add, [pb[2], bct], [a_])
            act(lg.ap, lg.ap, AF.Exp, [lg], [lg], scale=-1.0)
            act(lg.ap, lg.ap, AF.Ln, [lg], [lg], bias=1.0)
            act(lg.ap, lg.ap, AF.Exp, [lg], [lg], scale=-1.0)
            act(a_.ap, a_.ap, AF.Exp, [a_], [a_], scale=-1.0)
            act(a_.ap, a_.ap, AF.Ln, [a_], [a_], bias=1.0)
            act(a_.ap, a_.ap, AF.Exp, [a_], [a_], scale=-1.0)
            cp("act", vs[:, i, :], pc[:, 0:512], [pc], [vs])
            tt("dve", v3(kkn.ap), v3(kkn.ap), stK[:, 16:24].unsqueeze(2).to_broadcast([128, 8, 64]), ALU.mult, [kkn, stK], [kkn])
            ts("dve", lg.ap, lg.ap, -0.6065306597126334, None, ALU.mult, None, [lg], [lg])
            mm(pb[2][:, 0:512], mIN, lg.ap, True, True, [pkt, lg], [pb[2]])
            cl = S_[5]
            cp("act", cl.ap, pb[2][:, 0:512], [pb[2]], [cl])
            km = S_[9]
            ts("dve", km.ap, a_.ap, -1.0, None, ALU.add, None, [a_], [km])
            tt("dve", km.ap, km.ap, bct[:, BC_KA:BC_KA + 512], ALU.mult, [km, bct], [km])
            ts("dve", km.ap, km.ap, 1.0, None, ALU.add, None, [km], [km])
            tt("dve", km.ap, km.ap, k_.ap, ALU.mult, [km, k_], [km])
            dump(lg, 2048, row0)
            dump(a_, 2560, row0)
            dump(g_, 3072, row0)
            dump(cl, 3584, row0)
            dump(kkn, 4096, row0)
            dump(km, 4608, row0)
            dump(r_, 5632, row0)
            dump(k_, 6144, row0)
            dump(v_, 6656, row0)
            for h in range(8):
                mm(pc[0:64, h:h + 1], cl[:, h * 64:(h + 1) * 64], pkt[:, PK_ELAST:PK_ELAST + 1], True, True, [cl, pkt], [pc])
            eA, eN, eP = S_[10], S_[3], S_[4]
            tt("dve", eA.ap, cl.ap, lg.ap, ALU.subtract, [cl, lg], [eA])
            act(eA.ap, eA.ap, AF.Exp, [eA], [eA])
            act(eN.ap, cl.ap, AF.Exp, [cl], [eN], scale=-1.0)
            act(eP.ap, cl.ap, AF.Exp, [cl], [eP])
            act(gC[0:64, :], pc[0:64, 0:8], AF.Exp, [pc], [gC])
            stt("dve", Atc.ap, kkn.ap, -1.0, eA.ap, ALU.mult, ALU.mult, [kkn, eA], [Atc])
            cp("dve", At[:, :, 0:64], v3(Atc.ap), [Atc], [At])
            tt("dve", a_.ap, kkn.ap, a_.ap, ALU.mult, [kkn, a_], [a_])
            tt("dve", Bt.ap, a_.ap, eN.ap, ALU.mult, [a_, eN], [Bt])
            tt("dve", Kt.ap, km.ap, eN.ap, ALU.mult, [km, eN], [Kt])
            tt("dve", Rt.ap, r_.ap, eP.ap, ALU.mult, [r_, eP], [Rt])
            bon = S_[10]
            tt("dve", S_[3].ap, r_.ap, km.ap, ALU.mult, [r_, km], [S_[3]])
            tt("dve", S_[3].ap, S_[3].ap, bct[:, BC_RK:BC_RK + 512], ALU.mult, [S_[3], bct], [S_[3]])
            red("dve", stB[:, 0:8], v3(S_[3].ap), ALU.add, [S_[3]], [stB])
            tt("dve", v3(bon.ap), v3(v_.ap), stB[:, 0:8].unsqueeze(2).to_broadcast([128, 8, 64]), ALU.mult, [v_, stB], [bon])

            print("MARK fmajor", P.nadd, flush=True)
            for hp in range(4):
                tr(pT[:, hp * 128:(hp + 1) * 128], Atc[:, hp * 128:(hp + 1) * 128], identb, [Atc, cb], [pT])
                tr(pT[:, 512 + hp * 128:512 + (hp + 1) * 128], Rt[:, hp * 128:(hp + 1) * 128], identb, [Rt, cb], [pT])
            cp("act", ART[:, :, 0, :], pT[:, 0:512].rearrange("p (a t) -> p a t", a=4), [pT], [ART])
            cp("act", ART[:, :, 1, :], pT[:, 512:1024].rearrange("p (a t) -> p a t", a=4), [pT], [ART])
            for hp in range(4):
                tr(pT[:, hp * 128:(hp + 1) * 128], Bt[:, hp * 128:(hp + 1) * 128], identb, [Bt, cb], [pT])
                tr(pT[:, 512 + hp * 128:512 + (hp + 1) * 128], Kt[:, hp * 128:(hp + 1) * 128], identb, [Kt, cb], [pT])
            cp("act", BT.ap, pT[:, 0:512].rearrange("p (a t) -> p a t", a=4), [pT], [BT])
            cp("act", KTt.ap, pT[:, 512:1024].rearrange("p (a t) -> p a t", a=4), [pT], [KTt])

            N0, L0 = Nn[0], Ll[0]
            for hg in range(2):
                for hh in range(4):
                    h = hg * 4 + hh
                    hp, b0 = h // 2, (h % 2) * 64
                    par, q = hh % 2, hh // 2
                    rhs2 = ART[b0:b0 + 64, hp, :, :]
                    o1 = par * 512 + q * 256
                    o3 = par * 512 + q * 128
                    mm(pb[0][:, o1:o1 + 256], BT[b0:b0 + 64, hp, :], rhs2, True, True, [BT, ART], [pb[0]])
                    mm(pb[1][:, o1:o1 + 256], KTt[b0:b0 + 64, hp, :], rhs2, True, True, [KTt, ART], [pb[1]])
                    mm(pb[2][:, o3:o3 + 128], ART[b0:b0 + 64, hp, 0, :], BT[b0:b0 + 64, hp, :], True, True, [BT, ART], [pb[2]])
                g1 = pb[0].ap.rearrange("p (par q two t) -> p par q two t", par=2, q=2, two=2)
                g2 = pb[1].ap.rearrange("p (par q two t) -> p par q two t", par=2, q=2, two=2)
                g3 = pb[2].ap.rearrange("p (par x) -> p par x", par=2)
                bSU = mSU.unsqueeze(1).to_broadcast([128, 2, 128])
                bIN = mIN.unsqueeze(1).to_broadcast([128, 2, 128])
                bLO = mLO.unsqueeze(1).to_broadcast([128, 2, 128])
                hv = lambda T_: T_.ap.rearrange("p (g q par) t -> p g par q t", g=2, q=2, par=2)
                for par in range(2):
                    tt("dve", hv(N0)[:, hg, par], g1[:, par, :, 0, :], bSU, ALU.mult, [pb[0], pkt], [N0])
                    tt("dve", hv(MrbT)[:, hg, par], g1[:, par, :, 1, :], bIN, ALU.mult, [pb[0], pkt], [MrbT])
                    tt("dve", hv(LakT)[:, hg, par], g2[:, par, :, 0, :], bSU, ALU.mult, [pb[1], pkt], [LakT])
                    tt("dve", hv(MrkT)[:, hg, par], g2[:, par, :, 1, :], bIN, ALU.mult, [pb[1], pkt], [MrkT])
                    tt("dve", hv(L0)[:, hg, par], g3[:, par, 0:256].rearrange("p (q t) -> p q t", q=2), bLO, ALU.mult, [pb[2], pkt], [L0])
            for h in range(8):
                mm(pc[:, h * 64:(h + 1) * 64], LakT[:, h, :], Vb[:, h * 64:(h + 1) * 64], True, True, [LakT, Vb], [pc])
            cp("act", At[:, :, 64:128], pc.ap.rearrange("p (h d) -> p h d", h=8), [pc], [At])
            print("MARK neumann", P.nadd, flush=True)
            Ycur = At
            Ncur, Lcur = N0, L0
            for bk in range(2):
                mm(pb[2][:, bk * 512:(bk + 1) * 512], identb, At[:, 4 * bk:4 * bk + 4, :], True, False, [cb, At], [pb[2]])
            hst_ = head_steps(tg + 1) if tg + 1 < NSEQ * NT else []
            for lvl in range(7):
                for h in range(8):
                    mm(pb[2][:, h * 128:(h + 1) * 128], Ncur[:, h, :], Ycur[:, h, :], False, lvl == 6, [Ncur, Ycur], [pb[2]])
                if lvl < 6:
                    Nnx, Lnx = Nn[(lvl + 1) % 2], Ll[(lvl + 1) % 2]
                    for h in range(8):
                        mm(pb[0][:, h * 128:(h + 1) * 128], Lcur[:, h, :], Ncur[:, h, :], True, True, [Lcur, Ncur], [pb[0]])
                    if lvl < 5:
                        for h in range(8):
                            mm(pb[1][:, h * 128:(h + 1) * 128], Ncur[:, h, :], Lcur[:, h, :], True, True, [Lcur, Ncur], [pb[1]])
                    cp("act", Yb.ap, pb[2].ap.rearrange("p (h t) -> p h t", h=8), [pb[2]], [Yb])
                    cp("dve", Nnx.ap, pb[0].ap.rearrange("p (h t) -> p h t", h=8), [pb[0]], [Nnx])
                    if lvl < 5:
                        cp("dve", Lnx.ap, pb[1].ap.rearrange("p (h t) -> p h t", h=8), [pb[1]], [Lnx])
                    Ycur, Ncur, Lcur = Yb, Nnx, Lnx
                if lvl < len(hst_):
                    hst_[lvl]()
            cp("act", Yb.ap, pb[2].ap.rearrange("p (h t) -> p h t", h=8), [pb[2]], [Yb])
            AU = Yb
            print("MARK rhat", P.nadd, flush=True)
            for h in range(8):
                mm(pb[0][0:64, h * 128:(h + 1) * 128], AU[:, h, 0:64], MrbT[:, h, :], True, False, [AU, MrbT], [pb[0]])
                mm(pb[0][0:64, h * 128:(h + 1) * 128], Rt[:, h * 64:(h + 1) * 64], identb, False, True, [Rt, cb], [pb[0]])
            cp("act", RhT[0:64, :, :], pb[0][0:64, :].rearrange("p (h t) -> p h t", h=8), [pb[0]], [RhT])
            for h in range(8):
                mm(pb[1][0:64, h * 64:(h + 1) * 64], AU[:, h, 0:64], Bt[:, h * 64:(h + 1) * 64], True, True, [AU, Bt], [pb[1]])
                mm(pb[1][0:64, 512 + h * 64:512 + (h + 1) * 64], Bt[:, h * 64:(h + 1) * 64], AU[:, h, 64:128], True, False, [AU, Bt], [pb[1]])
                mm(pb[1][0:64, 512 + h * 64:512 + (h + 1) * 64], Kt[:, h * 64:(h + 1) * 64], Vb[:, h * 64:(h + 1) * 64], False, True, [Kt, Vb], [pb[1]])
            tt("dve", PhiT[0:64, :, :], pb[1][0:64, 0:512].rearrange("p (h d) -> p h d", h=8),
               pkt[0:64, PK_ID:PK_ID + 64].unsqueeze(1).to_broadcast([64, 8, 64]), ALU.add, [pb[1], pkt], [PhiT])
            tt("dve", Psig[0:64, :, :], pb[1][0:64, 512:1024].rearrange("p (h d) -> p h d", h=8),
               gC[0:64, :].unsqueeze(2).to_broadcast([64, 8, 64]), ALU.mult, [pb[1], gC], [Psig])
            for h in range(8):
                mm(pc[:, h * 64:(h + 1) * 64], RhT[0:64, h, :], Hb[0:64, h, :], True, False, [RhT, Hb], [pc])
                mm(pc[:, h * 64:(h + 1) * 64], MrbT[:, h, :], AU[:, h, 64:128], False, False, [MrbT, AU], [pc])
                mm(pc[:, h * 64:(h + 1) * 64], MrkT[:, h, :], Vb[:, h * 64:(h + 1) * 64], False, True, [MrkT, Vb], [pc])
            orw = S_[3]
            cp("act", orw.ap, pc[:, 0:512], [pc], [orw])
            dump(orw, 5120, row0)
            for h in range(8):
                mm(pb[0][0:64, h * 64:(h + 1) * 64], PhiT[0:64, h, :], Hst[0:64, h, :], True, True, [PhiT, Hst], [pb[0]])
            tt("dve", Hst[0:64, :, :], pb[0][0:64, 0:512].rearrange("p (h d) -> p h d", h=8),
               gC[0:64, :].unsqueeze(2).to_broadcast([64, 8, 64]), ALU.mult, [pb[0], gC], [Hst])
            tt("dve", Hst[0:64, :, :], Hst[0:64, :, :], Psig[0:64, :, :], ALU.add, [Hst, Psig], [Hst])
            cp("dve", Hb[0:64, :, :], Hst[0:64, :, :], [Hst], [Hb])
            red("dve", st8[:, 40:48], v3(orw.ap), ALU.add, [orw], [st8])
            ts("dve", st8[:, 40:48], st8[:, 40:48], 1.0 / 64, None, ALU.mult, None, [st8], [st8])
            tt("dve", v3(orw.ap), v3(orw.ap), st8[:, 40:48].unsqueeze(2).to_broadcast([128, 8, 64]), ALU.subtract, [orw, st8], [orw])
            tt("dve", S_[4].ap, orw.ap, orw.ap, ALU.mult, [orw], [S_[4]])
            red("dve", st8[:, 48:56], v3(S_[4].ap), ALU.add, [S_[4]], [st8])
            ts("dve", st8[:, 48:56], st8[:, 48:56], 1.0 / 64, 64e-5, ALU.mult, ALU.add, [st8], [st8])
            act(st8[:, 56:64], st8[:, 48:56], AF.Ln, [st8], [st8])
            act(st8[:, 48:56], st8[:, 56:64], AF.Exp, [st8], [st8], scale=-0.5)
            tt("dve", v3(orw.ap), v3(orw.ap), st8[:, 48:56].unsqueeze(2).to_broadcast([128, 8, 64]), ALU.mult, [orw, st8], [orw])
            tt("dve", orw.ap, orw.ap, bct[:, BC_LNW:BC_LNW + 512], ALU.mult, [orw, bct], [orw])
            tt("dve", orw.ap, orw.ap, bct[:, BC_LNB:BC_LNB + 512], ALU.add, [orw, bct], [orw])
            tt("dve", orw.ap, orw.ap, bon.ap, ALU.add, [orw, bon], [orw])
            tt("dve", mix[:, 512:1024], orw.ap, g_.ap, ALU.mult, [orw, g_], [mix])

            print("MARK sb", P.nadd, flush=True)
            KSKIP = _os.environ.get("KSKIP", "")
            blk = lambda h: (h % 2) * 4 + h // 2

            h8 = lambda ap: ap.rearrange("p (h t) -> p h t", h=8)

            def sb_z(kb):
                pz = pb[kb % 2]
                for h in range(8):
                    hp, b0 = h // 2, (h % 2) * 64
                    mm(pz[:, blk(h) * 128:(blk(h) + 1) * 128], kT[b0:b0 + 64, hp, kb * 128:(kb + 1) * 128], qT[b0:b0 + 64, hp, :], True, True, [kT, qT], [pz])

            def sb_el(kb):
                par_ = kb % 2
                E_, Lp, pz = E2[par_], Lp2[par_], pb[par_]
                act(E_.ap, pz.ap, AF.Exp, [pz], [E_])
                if kb == i:
                    tt("dve", h8(E_.ap), h8(E_.ap), mSU.unsqueeze(1).to_broadcast([128, 8, 128]), ALU.mult, [E_, pkt], [E_])
                act(Lp.ap, E_.ap, AF.Ln, [E_], [Lp], bias=1.0)

            def sb_cum(kb):
                Lp = Lp2[kb % 2]
                mm(pb[2][:, 0:512], negtri, Lp[:, 0:512], True, False, [cb, Lp], [pb[2]])
                mm(pb[2][:, 512:1024], negtri, Lp[:, 512:1024], True, False, [cb, Lp], [pb[2]])
                for h in range(8):
                    hp, b0 = h // 2, (h % 2) * 64
                    mm(pb[2][:, blk(h) * 128:(blk(h) + 1) * 128], kT[b0:b0 + 64, hp, kb * 128:(kb + 1) * 128], qT[b0:b0 + 64, hp, :], False, True, [kT, qT], [pb[2]])
                if kb != kbs[0]:
                    for h in range(8):
                        mm(pTf[:, h:h + 1], Lp[:, blk(h) * 128:(blk(h) + 1) * 128], negtri[:, 0:1], True, True, [Lp, cb], [pTf])

            def sb_w(kb):
                ATt = AT2[kb % 2]
                act(ATt.ap, pb[2].ap, AF.Exp, [pb[2]], [ATt])
                if kb == i:
                    tt("dve", h8(ATt.ap), h8(ATt.ap), mSU.unsqueeze(1).to_broadcast([128, 8, 128]), ALU.mult, [ATt, pkt], [ATt])
                if kb != kbs[0]:
                    act(Fs.ap, pTf[:, 0:8], AF.Exp, [pTf], [Fs])

            def sb_pv(kb):
                ATt = AT2[kb % 2]
                for h in range(8):
                    mm(pc[:, h * 64:(h + 1) * 64], ATt[:, blk(h) * 128:(blk(h) + 1) * 128], vs[:, kb, h * 64:(h + 1) * 64], True, True, [ATt, vs], [pc])
                if kb == kbs[0]:
                    cp("dve", acc.ap, pc[:, 0:512], [pc], [acc])
                else:
                    tt("dve", v3(acc.ap), v3(acc.ap), Fs.ap.unsqueeze(2).to_broadcast([128, 8, 64]), ALU.mult, [acc, Fs], [acc])
                    tt("dve", acc.ap, acc.ap, pc[:, 0:512], ALU.add, [acc, pc], [acc])

            kbs = [kb for kb in range(i + 1) if not ("s" in KSKIP and kb > 0)]
            sb_z(kbs[0])
            sb_el(kbs[0])
            if len(kbs) > 1:
                sb_z(kbs[1])
            for n_, kb in enumerate(kbs):
                sb_cum(kb)
                if n_ + 2 < len(kbs):
                    sb_z(kbs[n_ + 2])
                if n_ + 1 < len(kbs):
                    sb_el(kbs[n_ + 1])
                sb_w(kb)
                sb_pv(kb)
            tt("dve", S_[4].ap, acc.ap, acc.ap, ALU.mult, [acc], [S_[4]])
            red("dve", st8[:, 40:48], v3(S_[4].ap), ALU.add, [S_[4]], [st8])
            ts("dve", st8[:, 40:48], st8[:, 40:48], 1.0 / 64, 1e-6, ALU.mult, ALU.add, [st8], [st8])
            act(st8[:, 56:64], st8[:, 40:48], AF.Ln, [st8], [st8])
            act(st8[:, 40:48], st8[:, 56:64], AF.Exp, [st8], [st8], scale=-0.5)
            tt("dve", v3(mix[:, 0:512]), v3(acc.ap), st8[:, 40:48].unsqueeze(2).to_broadcast([128, 8, 64]), ALU.mult, [acc, st8], [mix])

            print("MARK outproj", P.nadd, flush=True)
            for c in range(8):
                tr(pT[:, c * 128:(c + 1) * 128], mix[:, c * 128:(c + 1) * 128], identb, [mix, cb], [pT])
            cp("act", mixT.ap, pT.ap.rearrange("p (c t) -> p c t", c=8), [pT], [mixT])
            for half in range(2):
                for c in range(8):
                    mm(pb[0][:, half * 512:(half + 1) * 512], mixT[:, c, :], Wout[:, c, half * 512:(half + 1) * 512], c == 0, c == 7, [mixT, Wout], [pb[0]])
            ht = xtile
            tt("dve", ht.ap, pb[0].ap, xtile.ap, ALU.add, [pb[0], xtile], [ht])
            dma("sp", hbuf[row0:row0 + 128, :], ht.ap, [ht], [])
            if dbg:
                dma("sp", dbg_t[row0:row0 + 128, 0:1024], ht.ap, [ht], [], final=True)
            act(xn2.ap, ht.ap, AF.Square, [ht], [xn2, st8], accum=st8[:, 4:5])
            ts("dve", st8[:, 5:6], st8[:, 4:5], 1.0 / 1024, 1e-6, ALU.mult, ALU.add, [st8], [st8])
            act(st8[:, 6:7], st8[:, 5:6], AF.Ln, [st8], [st8])
            act(st8[:, 7:8], st8[:, 6:7], AF.Exp, [st8], [st8], scale=-0.5)
            stt("dve", xn2.ap, ht.ap, st8[:, 7:8], bct[:, BC_GFFN:BC_GFFN + 1024], ALU.mult, ALU.mult, [ht, st8, bct], [xn2])
            if tg + 1 < NSEQ * NT:
                rw_inproj(tg + 1)
            for c in range(8):
                tr(pT[:, c * 128:(c + 1) * 128], xn2[:, c * 128:(c + 1) * 128], identb, [xn2, cb], [pT])
            cp("act", xn2T.ap, pT.ap.rearrange("p (c t) -> p c t", c=8), [pT], [xn2T])
            dma("sp", x2d[:, :, row0:row0 + 128], xn2T.ap, [xn2T], [])
            for c in range(8):
                mm(pc[:, 0:36], xn2T[:, c, :], Wr[:, c, :], c == 0, c == 7, [xn2T, Wr], [pc])
            tt("dve", rl[:, 0:36], pc[:, 0:36], bct[:, BC_RB:BC_RB + 36], ALU.add, [pc, bct], [rl])
            red("dve", st8[:, 40:41], rl[:, 0:4], ALU.max, [rl], [st8])
            ts("dve", rtmp[:, 0:4], rl[:, 0:4], st8[:, 40:41], None, ALU.is_equal, None, [rl, st8], [rtmp])
            ts("dve", rtmp[:, 4:8], rl[:, 0:4], st8[:, 40:41], None, ALU.subtract, None, [rl, st8], [rtmp])
            act(rtmp[:, 4:8], rtmp[:, 4:8], AF.Exp, [rtmp], [rtmp, st8], accum=st8[:, 41:42])
            P.add("dve", lambda e: e.reciprocal(out=st8[:, 42:43], in_=st8[:, 41:42]), [st8], [st8])
            tt("dve", rtmp[:, 8:40].rearrange("p (g j) -> p g j", g=4), rl[:, 4:36].rearrange("p (g j) -> p g j", g=4),
               rtmp[:, 0:4].unsqueeze(2).to_broadcast([128, 4, 8]), ALU.mult, [rl, rtmp], [rtmp])
            red("dve", rtmp[:, 40:48], rtmp[:, 8:40].rearrange("p (g j) -> p j g", g=4), ALU.add, [rtmp], [rtmp])
            red("dve", st8[:, 43:44], rtmp[:, 40:48], ALU.max, [rtmp], [st8])
            ts("dve", rtmp[:, 48:56], rtmp[:, 40:48], st8[:, 43:44], None, ALU.is_equal, None, [rtmp, st8], [rtmp])
            stt("dve", rtmp[:, 56:64], rtmp[:, 48:56], -1e30, rtmp[:, 40:48], ALU.mult, ALU.add, [rtmp], [rtmp])
            red("dve", st8[:, 44:45], rtmp[:, 56:64], ALU.max, [rtmp], [st8])
            ts("dve", rtmp[:, 64:72], rtmp[:, 56:64], st8[:, 44:45], None, ALU.is_equal, None, [rtmp, st8], [rtmp])
            tt("dve", st8[:, 45:46], st8[:, 44:45], st8[:, 43:44], ALU.subtract, [st8], [st8])
            act(st8[:, 46:47], st8[:, 45:46], AF.Exp, [st8], [st8])
            ts("dve", st8[:, 47:48], st8[:, 46:47], 1.0, None, ALU.add, None, [st8], [st8])
            P.add("dve", lambda e: e.reciprocal(out=st8[:, 47:48], in_=st8[:, 47:48]), [st8], [st8])
            tt("dve", st8[:, 47:48], st8[:, 47:48], st8[:, 42:43], ALU.mult, [st8], [st8])
            tt("dve", st8[:, 46:47], st8[:, 46:47], st8[:, 47:48], ALU.mult, [st8], [st8])
            ts("dve", rtmp[:, 72:80], rtmp[:, 48:56], st8[:, 47:48], None, ALU.mult, None, [rtmp, st8], [rtmp])
            stt("dve", rtmp[:, 72:80], rtmp[:, 64:72], st8[:, 46:47], rtmp[:, 72:80], ALU.mult, ALU.add, [rtmp, st8], [rtmp])
            tt("dve", call[:, tg, :].rearrange("p (g j) -> p g j", g=4), rtmp[:, 0:4].unsqueeze(2).to_broadcast([128, 4, 8]),
               rtmp[:, 72:80].unsqueeze(1).to_broadcast([128, 4, 8]), ALU.mult, [rtmp], [call])
            if dbg:
                cp("dve", E_.ap, mix.ap, [mix], [E_])
                dma("sp", dbg_t[row0:row0 + 128, 1024:2048], E_.ap, [E_], [], final=True)

    print("MARK moe", P.nadd, flush=True)
    if _os.environ.get("KNOMOE"):
        P.stopped = True
    P.barrier()
    ar.off = mark_moe
    TB = min(2048, NTOK)
    NPASS = NTOK // TB
    TPB = TB // 128
    gfin = A([1024], F32)
    dma("sp", gfin.ap, bc[:, BC_GFIN:BC_GFIN + 1024], [], [gfin])
    x2 = A([8, TB], BF16)
    yh = []
    yacc = []
    for _t in range(TPB):
        h0_ = A([512], F32)
        at0 = ar.last_at
        h1_ = A([512], F32)
        yh.append((h0_, h1_))
        yacc.append(A([1024], F32, at=at0, toks=h0_.toks + h1_.toks))
    NWB = 2
    mstg = [A([2048], F32) for _ in range(4)]
    mi = 0
    Wg_ = [A([8, 256], BF16) for _ in range(NWB)]
    Wu_ = [A([8, 256], BF16) for _ in range(NWB)]
    Wd_ = [A([2, 1024], BF16) for _ in range(NWB)]
    hT = [A([2, TB], BF16) for _ in range(2)]
    sgt = [A([512], F32) for _ in range(2)]
    ysct = [A([512], F32) for _ in range(2)]
    st9 = A([8], F32)
    junk2 = A([1024], F32)
    ot = [A([1024], F32) for _ in range(2)]
    wi = 0
    for ps_ in range(NPASS):
        tok0 = ps_ * TB
        dma("sp", x2.ap, x2d[:, :, tok0:tok0 + TB], [], [x2])
        for t in range(TPB):
            dma("sp", yacc[t].ap, hbuf[tok0 + t * 128:tok0 + (t + 1) * 128, :], [], [yacc[t]])
        mi_ = [mi]
        k2_ = [0]
        yi_ = [0]
        tokY0, tokY1 = Tok(), Tok()

        def moe_load(ex):
            wb = ex % NWB
            for (dst, src, na_) in ((Wg_[wb], wg[ex], 8), (Wu_[wb], wu[ex], 8), (Wd_[wb], wd[ex], 2)):
                sgm = mstg[mi_[0] % len(mstg)]
                mi_[0] += 1
                sv = sgm.ap.rearrange("p (a b) -> p a b", a=na_)
                dma("sp", sv, src.rearrange("(c p) n -> p c n", p=128), [], [sgm])
                cp("pool", dst.ap, sv, [sgm], [dst])

        def moe_gu(ex):
            wb = ex % NWB
            hTe = hT[ex % 2]
            NB = min(512, TB)
            for hc in range(2):
                for tb in range(TB // NB):
                    pgu = pb[k2_[0] % 2]
                    sg_ = sgt[k2_[0] % 2]
                    k2_[0] += 1
                    for c in range(8):
                        mm(pgu[:, 0:NB], Wg_[wb][:, c, hc * 128:(hc + 1) * 128], x2[:, c, tb * NB:(tb + 1) * NB], c == 0, c == 7, [Wg_[wb], x2], [pgu])
                    for c in range(8):
                        mm(pgu[:, 512:512 + NB], Wu_[wb][:, c, hc * 128:(hc + 1) * 128], x2[:, c, tb * NB:(tb + 1) * NB], c == 0, c == 7, [Wu_[wb], x2], [pgu])
                    act(sg_[:, 0:NB], pgu[:, 0:NB], AF.Silu, [pgu], [sg_])
                    tt("dve", hTe[:, hc, tb * NB:(tb + 1) * NB], sg_[:, 0:NB], pgu[:, 512:512 + NB], ALU.mult, [sg_, pgu], [hTe])

        yring = [(pb[2][:, 0:512], Tile(pb[2][:, 0:512], [tokY0])), (pb[2][:, 512:1024], Tile(pb[2][:, 512:1024], [tokY1])),
                 (pc[:, 0:512], pc), (pTf[:, 0:512], pTf)]

        def moe_down(ex):
            wb = ex % NWB
            hTe = hT[ex % 2]
            for t in range(TPB):
                tgl = tok0 // 128 + t
                for half in range(2):
                    yp, ytok = yring[yi_[0] % 4]
                    yi_[0] += 1
                    for hc in range(2):
                        mm(yp, hTe[:, hc, t * 128:(t + 1) * 128], Wd_[wb][:, hc, half * 512:(half + 1) * 512],
                           hc == 0, hc == 1, [hTe, Wd_[wb]], [ytok])
                    stt("dve", yh[t][half].ap, yp, call[:, tgl, ex:ex + 1], yh[t][half].ap, ALU.mult, ALU.add,
                        [ytok, call, yh[t][half]], [yh[t][half]])

        moe_load(0)
        moe_load(1)
        moe_gu(0)
        for ex in range(32):
            if ex + 1 < 32:
                moe_gu(ex + 1)
            moe_down(ex)
            if ex + 2 < 32:
                moe_load(ex + 2)
        mi = mi_[0]
        for t in range(TPB):
            o_ = ot[t % 2]
            act(junk2.ap, yacc[t].ap, AF.Square, [yacc[t]], [junk2, st9], accum=st9[:, 0:1])
            ts("dve", st9[:, 1:2], st9[:, 0:1], 1.0 / 1024, 1e-6, ALU.mult, ALU.add, [st9], [st9])
            act(st9[:, 2:3], st9[:, 1:2], AF.Ln, [st9], [st9])
            act(st9[:, 3:4], st9[:, 2:3], AF.Exp, [st9], [st9], scale=-0.5)
            stt("dve", o_.ap, yacc[t].ap, st9[:, 3:4], gfin.ap, ALU.mult, ALU.mult, [yacc[t], st9, gfin], [o_])
            dma("sp", out[tok0 + t * 128:tok0 + (t + 1) * 128, :], o_.ap, [o_], [], final=True)
    stats = P.emit()
    print("ops", stats, "sbuf hi bytes", ar.hi * 2, flush=True)
    return nc


_CACHE = {}


def _pack(inp):
    f = np.float32
    g = lambda k: np.asarray(inp[k], dtype=f)
    pk = np.zeros((128, NPK), f)
    pk[:, PK_GMIX:PK_GMIX + 8] = g("norm_mix_g")[0].reshape(8, 128).T
    pk[:, PK_SBG:PK_SBG + 4] = g("sb_out_g")[0].reshape(4, 128).T
    mu = g("shift_mu")[0]
    pk[:, PK_MUL] = mu[1536:1664]
    pk[:, PK_MUL + 1] = mu[1664:1792]
    pk[127, PK_ELAST] = 1.0
    idx = np.arange(128)
    pk[:, PK_ID:PK_ID + 128] = np.eye(128, dtype=f)
    pk[:, PK_SU:PK_SU + 128] = (idx[:, None] < idx[None, :]).astype(f)
    pk[:, PK_IN:PK_IN + 128] = (idx[:, None] <= idx[None, :]).astype(f)
    pk[:, PK_LO:PK_LO + 128] = (idx[None, :] < idx[:, None]).astype(f)
    pk[:, PK_NT:PK_NT + 128] = -(idx[:, None] >= idx[None, :]).astype(f)
    row = np.zeros((NBC,), f)
    row[BC_MU:BC_MU + 1536] = mu[0:1536]
    row[BC_W0:BC_W0 + 512] = g("rw_w0")[0]
    row[BC_A0:BC_A0 + 512] = g("rw_a0")[0]
    row[BC_KK:BC_KK + 512] = g("rw_k_k")[0]
    row[BC_KA:BC_KA + 512] = g("rw_k_a")[0]
    row[BC_RK:BC_RK + 512] = g("rw_r_k")[0].reshape(512)
    row[BC_LNW:BC_LNW + 512] = g("rw_ln_w")[0]
    row[BC_LNB:BC_LNB + 512] = g("rw_ln_b")[0]
    row[BC_GFFN:BC_GFFN + 1024] = g("norm_ffn_g")[0]
    row[BC_RB:BC_RB + 4] = g("router_grp_b")[0]
    row[BC_RB + 4:BC_RB + 36] = g("router_exp_b")[0]
    row[BC_GFIN:BC_GFIN + 1024] = g("final_norm_g")
    bcm = np.ascontiguousarray(np.broadcast_to(row[None, :], (128, NBC)))
    lwm = np.concatenate([np.concatenate([g("rw_w2")[0], g("rw_a2")[0]], axis=0), g("rw_g2")[0]], axis=1)
    wrm = np.concatenate([g("router_grp_w")[0], g("router_exp_w")[0]], axis=1)
    return dict(w_in=g("w_in")[0], w_out=g("w_out")[0], wr=np.ascontiguousarray(wrm), lw=np.ascontiguousarray(lwm),
                wg=g("exp_w_gate")[0], wu=g("exp_w_up")[0], wd=g("exp_w_down")[0], pk=pk, bc=bcm)


def run(inputs, ncores, dbg=False):
    x = np.asarray(inputs["x"], dtype=np.float32)
    B, S, D = x.shape
    nseq = B // ncores
    key = (S, nseq, dbg)
    if key not in _CACHE:
        _CACHE[key] = build(S, nseq, dbg)
    nc = _CACHE[key]
    shared = _pack(inputs)
    in_maps = []
    for c in range(ncores):
        m = dict(shared)
        m["x"] = np.ascontiguousarray(x[c * nseq:(c + 1) * nseq].reshape(nseq * S, D))
        in_maps.append(m)
    res = run_bass_kernel_spmd(nc, in_maps, core_ids=list(range(ncores)))
    out = np.stack([r["out"].reshape(nseq, S, D) for r in res.results], axis=0).reshape(B, S, D)
    if dbg:
        return out, [r["dbg"] for r in res.results]
    return out


def kernel(**inputs):
    return run(inputs, 8).astype(np.float32)
```
